# Optimizing a Trainium2 kernel written in Bass

```python
import math
import jax, jax.numpy as jnp
from jax import lax
import numpy as np

D_MODEL = 1024
BATCH = 32
SEQ = 2048
DEPTH = 4

RET_HEADS = 4
RET_QK_DIM = 32
RET_V_DIM = 64
RET_CHUNK = 128
RET_QK_WIDTH = RET_HEADS * RET_QK_DIM
RET_V_WIDTH = RET_HEADS * RET_V_DIM
ROPE_BASE = 10000.0
GMLP_GROUPS = 4
GMLP_GROUP_CH = 64
GMLP_WIDTH = GMLP_GROUPS * GMLP_GROUP_CH
GMLP_CHUNK = 128
CONV_WIDTH = 256
CONV_K = 3
N_BRANCH = 3
BRANCH_WIDTH = 256
GATE_RANK = 128
IN_SPLITS = [RET_QK_WIDTH, RET_QK_WIDTH, RET_V_WIDTH, RET_V_WIDTH,
             GMLP_WIDTH, GMLP_WIDTH,
             CONV_WIDTH, CONV_WIDTH, CONV_WIDTH,
             N_BRANCH * GATE_RANK]
IN_WIDTH = sum(IN_SPLITS)
N_EXPERTS = 32
TOP_K = 4
D_FF = 256
SWIGLU_LIMIT = 7.0
SWIGLU_ALPHA = 1.702
MOE_BLOCK = 512
DEEPNORM_ALPHA = (2.0 * DEPTH) ** 0.25
DEEPNORM_BETA = (8.0 * DEPTH) ** -0.25
LN_EPS = 1e-5

kernel_name = "hybrid_retention_gmlp_shortconv_moe_deepnorm"


def _layernorm(x, g, b):
    xf = x.astype(jnp.float32)
    mu = jnp.mean(xf, axis=-1, keepdims=True)
    var = jnp.mean(jnp.square(xf - mu), axis=-1, keepdims=True)
    y = (xf - mu) * lax.rsqrt(var + LN_EPS) * g.astype(jnp.float32) + b.astype(jnp.float32)
    return y.astype(x.dtype)


def _rotary(t, positions):
    dh = t.shape[-1]
    inv_freq = ROPE_BASE ** (-jnp.arange(0, dh, 2, dtype=jnp.float32) / dh)
    ang = positions.astype(jnp.float32)[..., None] * inv_freq
    cos = jnp.cos(ang)[:, :, None, :]
    sin = jnp.sin(ang)[:, :, None, :]
    t1, t2 = jnp.split(t, 2, axis=-1)
    return jnp.concatenate([t1 * cos - t2 * sin, t2 * cos + t1 * sin], axis=-1)


def _retention(q, k, v, g, positions):
    out_dtype = v.dtype
    bsz, seq, _ = q.shape
    n_chunks = seq // RET_CHUNK
    f32 = jnp.float32
    q = _rotary(q.astype(f32).reshape(bsz, seq, RET_HEADS, RET_QK_DIM), positions)
    k = _rotary(k.astype(f32).reshape(bsz, seq, RET_HEADS, RET_QK_DIM), positions) * (RET_QK_DIM ** -0.5)
    v = v.astype(f32).reshape(bsz, seq, RET_HEADS, RET_V_DIM)
    qc = q.reshape(bsz, n_chunks, RET_CHUNK, RET_HEADS, RET_QK_DIM)
    kc = k.reshape(bsz, n_chunks, RET_CHUNK, RET_HEADS, RET_QK_DIM)
    vc = v.reshape(bsz, n_chunks, RET_CHUNK, RET_HEADS, RET_V_DIM)

    log_gamma = jnp.log(1.0 - 2.0 ** (-5.0 - jnp.arange(RET_HEADS, dtype=f32)))
    idx = jnp.arange(RET_CHUNK, dtype=f32)
    diff = idx[:, None] - idx[None, :]
    decay_mask = jnp.where(diff[None] >= 0,
                           jnp.exp(log_gamma[:, None, None] * jnp.maximum(diff, 0.0)[None]),
                           0.0)
    scores = jnp.einsum('bnchk,bnmhk->bnhcm', qc, kc) * decay_mask[None, None]
    o_intra = jnp.einsum('bnhcm,bnmhv->bnchv', scores, vc)
    zeta = jnp.exp(log_gamma[:, None] * (RET_CHUNK - 1.0 - idx)[None])
    kv = jnp.einsum('bnmhk,bnmhv,hm->bnhkv', kc, vc, zeta)
    chunk_decay = jnp.exp(log_gamma * RET_CHUNK)[None, :, None, None]
    kv_t = jnp.moveaxis(kv, 1, 0)

    def step(state, kv_n):
        return state * chunk_decay + kv_n, state

    _, r_prev = lax.scan(step, jnp.zeros_like(kv_t[0]), kv_t)
    r_prev = jnp.moveaxis(r_prev, 0, 1)
    xi = jnp.exp(log_gamma[:, None] * (idx + 1.0)[None]).T
    o_cross = jnp.einsum('bnchk,bnhkv->bnchv', qc, r_prev) * xi[None, None, :, :, None]
    o = (o_intra + o_cross).reshape(bsz, seq, RET_HEADS, RET_V_DIM)
    mu = jnp.mean(o, axis=-1, keepdims=True)
    var = jnp.mean(jnp.square(o - mu), axis=-1, keepdims=True)
    o = ((o - mu) * lax.rsqrt(var + LN_EPS)).reshape(bsz, seq, RET_V_WIDTH)
    return (jax.nn.silu(g.astype(f32)) * o).astype(out_dtype)


def _gmlp_spatial(zu, zv, ln_g, ln_b, w_spatial, b_spatial):
    bsz, seq, _ = zu.shape
    n_chunks = seq // GMLP_CHUNK
    u = jax.nn.gelu(zu)
    v = _layernorm(jax.nn.gelu(zv), ln_g, ln_b)
    vc = v.reshape(bsz, n_chunks, GMLP_CHUNK, GMLP_GROUPS, GMLP_GROUP_CH)
    causal = jnp.tril(jnp.ones((GMLP_CHUNK, GMLP_CHUNK), dtype=w_spatial.dtype))
    w_masked = w_spatial * causal[None]
    z = jnp.einsum('gcm,bnmgx->bncgx', w_masked, vc) + b_spatial.T[None, None, :, :, None]
    return u * z.reshape(bsz, seq, GMLP_WIDTH).astype(u.dtype)


def _short_conv(gate_b, gate_c, xin, conv_w):
    z = gate_c * xin
    rhs = conv_w.astype(z.dtype)[:, None, :]
    conv = lax.conv_general_dilated(z, rhs, window_strides=(1,), padding=[(CONV_K - 1, 0)],
                                    dimension_numbers=('NWC', 'WIO', 'NWC'),
                                    feature_group_count=CONV_WIDTH)
    return gate_b * conv


def _hybrid_mixer(h, positions, w_in, gmlp_ln_g, gmlp_ln_b, w_spatial, b_spatial,
                  conv_w, w_branch, w_gate_up, b_gate, w_out):
    bsz, seq, _ = h.shape
    proj = h @ w_in
    offs = [int(o) for o in np.cumsum(IN_SPLITS)[:-1]]
    (rq, rk, rv, rg, gu, gv, cb, cc, cx, gate_code) = jnp.split(proj, offs, axis=-1)
    r = _retention(rq, rk, rv, rg, positions)
    s = _gmlp_spatial(gu, gv, gmlp_ln_g, gmlp_ln_b, w_spatial, b_spatial)
    k = _short_conv(cb, cc, cx, conv_w)
    branches = jnp.stack([r, s.astype(r.dtype), k.astype(r.dtype)], axis=2)
    y = jnp.einsum('bsnw,nwd->bsnd', branches, w_branch)
    gate_code = gate_code.reshape(bsz, seq, N_BRANCH, GATE_RANK)
    g = jax.nn.sigmoid(jnp.einsum('bsnr,nrd->bsnd', gate_code, w_gate_up) + b_gate)
    merged = jnp.sum(g * y, axis=2)
    return merged @ w_out


def _moe(h, w_router, b_router, w_gu, b_gu, w_down, b_down):
    bsz, seq, d = h.shape
    n_tok = bsz * seq
    n_asg = n_tok * TOP_K
    n_blocks = n_asg // MOE_BLOCK + N_EXPERTS
    n_rows = n_blocks * MOE_BLOCK
    ht = h.reshape(n_tok, d)
    logits = (ht @ w_router + b_router).astype(jnp.float32)
    top_v, top_i = lax.top_k(logits, TOP_K)
    weights = jax.nn.softmax(top_v, axis=-1)
    flat_e = top_i.reshape(-1).astype(jnp.int32)
    order = jnp.argsort(flat_e)
    e_sorted = flat_e[order]
    tok_sorted = (order // TOP_K).astype(jnp.int32)
    w_sorted = weights.reshape(-1)[order]
    sizes = jnp.bincount(flat_e, length=N_EXPERTS).astype(jnp.int32)
    start = jnp.cumsum(sizes) - sizes
    pad_sizes = (sizes + MOE_BLOCK - 1) // MOE_BLOCK * MOE_BLOCK
    pad_end = jnp.cumsum(pad_sizes)
    pad_start = pad_end - pad_sizes
    dest = pad_start[e_sorted] + jnp.arange(n_asg, dtype=jnp.int32) - start[e_sorted]
    row_tok = jnp.full((n_rows,), n_tok, dtype=jnp.int32).at[dest].set(tok_sorted)
    row_w = jnp.zeros((n_rows,), jnp.float32).at[dest].set(w_sorted)
    block_start = jnp.arange(n_blocks, dtype=jnp.int32) * MOE_BLOCK
    block_e = jnp.minimum(jnp.searchsorted(pad_end, block_start, side='right'),
                          N_EXPERTS - 1).astype(jnp.int32)
    h_pad = jnp.concatenate([ht, jnp.zeros((1, d), ht.dtype)], axis=0)
    xs = h_pad[row_tok].reshape(n_blocks, MOE_BLOCK, d).astype(w_gu.dtype)
    gu = jnp.einsum('nmd,ndf->nmf', xs, w_gu[block_e]) + b_gu[block_e][:, None, :]
    gate = jnp.minimum(gu[..., :D_FF], SWIGLU_LIMIT)
    up = jnp.clip(gu[..., D_FF:], -SWIGLU_LIMIT, SWIGLU_LIMIT)
    act = (up + 1.0) * (gate * jax.nn.sigmoid(SWIGLU_ALPHA * gate))
    out = jnp.einsum('nmf,nfd->nmd', act.astype(w_down.dtype), w_down[block_e]) + b_down[block_e][:, None, :]
    out = out.reshape(n_rows, d).astype(jnp.float32) * row_w[:, None]
    y = jax.ops.segment_sum(out, row_tok, num_segments=n_tok + 1)[:n_tok]
    return y.reshape(bsz, seq, d).astype(h.dtype)


def setup_inputs(seed: int = 0) -> dict:
    key = jax.random.key(seed)
    ks = jax.random.split(key, 26)
    f32 = jnp.float32
    L, D, E, F = DEPTH, D_MODEL, N_EXPERTS, D_FF
    nrm = lambda k, shape, s: jax.random.normal(k, shape, f32) * s
    return {
        "x": nrm(ks[0], (BATCH, SEQ, D), 1.0),
        "c": nrm(ks[1], (BATCH, D), 1.0),
        "positions": jnp.broadcast_to(jnp.arange(SEQ, dtype=jnp.int32), (BATCH, SEQ)),
        "w_ada": nrm(ks[2], (L, D, 6 * D), 0.5 * D ** -0.5),
        "b_ada": nrm(ks[3], (L, 6 * D), 0.01),
        "w_in": nrm(ks[4], (L, D, IN_WIDTH), D ** -0.5),
        "gmlp_ln_g": 1.0 + nrm(ks[5], (L, GMLP_WIDTH), 0.01),
        "gmlp_ln_b": nrm(ks[6], (L, GMLP_WIDTH), 0.01),
        "w_spatial": nrm(ks[7], (L, GMLP_GROUPS, GMLP_CHUNK, GMLP_CHUNK), GMLP_CHUNK ** -0.5),
        "b_spatial": 1.0 + nrm(ks[8], (L, GMLP_GROUPS, GMLP_CHUNK), 0.01),
        "conv_w": nrm(ks[9], (L, CONV_K, CONV_WIDTH), CONV_K ** -0.5),
        "w_branch": nrm(ks[10], (L, N_BRANCH, BRANCH_WIDTH, D), DEEPNORM_BETA * BRANCH_WIDTH ** -0.5),
        "w_gate_up": nrm(ks[11], (L, N_BRANCH, GATE_RANK, D), GATE_RANK ** -0.5),
        "b_gate": nrm(ks[12], (L, N_BRANCH, D), 0.01),
        "w_out": nrm(ks[13], (L, D, D), DEEPNORM_BETA * D ** -0.5),
        "ln1_g": 1.0 + nrm(ks[14], (L, D), 0.01),
        "ln1_b": nrm(ks[15], (L, D), 0.01),
        "w_router": nrm(ks[16], (L, D, E), D ** -0.5),
        "b_router": nrm(ks[17], (L, E), 0.01),
        "w_gu": nrm(ks[18], (L, E, D, 2 * F), D ** -0.5),
        "b_gu": nrm(ks[19], (L, E, 2 * F), 0.01),
        "w_down": nrm(ks[20], (L, E, F, D), DEEPNORM_BETA * F ** -0.5),
        "b_down": nrm(ks[21], (L, E, D), 0.01),
        "ln2_g": 1.0 + nrm(ks[22], (L, D), 0.01),
        "ln2_b": nrm(ks[23], (L, D), 0.01),
    }


def reference(x, c, positions, w_ada, b_ada, w_in, gmlp_ln_g, gmlp_ln_b, w_spatial, b_spatial,
              conv_w, w_branch, w_gate_up, b_gate, w_out, ln1_g, ln1_b, w_router, b_router,
              w_gu, b_gu, w_down, b_down, ln2_g, ln2_b):
    out_dtype = x.dtype
    c_act = jax.nn.silu(c)
    for l in range(DEPTH):
        ada = c_act @ w_ada[l] + b_ada[l]
        sh1, sc1, gt1, sh2, sc2, gt2 = [a[:, None, :] for a in jnp.split(ada, 6, axis=-1)]
        h = x * (1.0 + sc1) + sh1
        mix = _hybrid_mixer(h, positions, w_in[l], gmlp_ln_g[l], gmlp_ln_b[l], w_spatial[l],
                            b_spatial[l], conv_w[l], w_branch[l], w_gate_up[l], b_gate[l], w_out[l])
        x = _layernorm(DEEPNORM_ALPHA * x + (1.0 + gt1) * mix, ln1_g[l], ln1_b[l])
        h2 = x * (1.0 + sc2) + sh2
        ffn = _moe(h2, w_router[l], b_router[l], w_gu[l], b_gu[l], w_down[l], b_down[l])
        x = _layernorm(DEEPNORM_ALPHA * x + (1.0 + gt2) * ffn, ln2_g[l], ln2_b[l])
    return x.astype(out_dtype)
```

```python
import contextlib
import math
import numpy as np
import concourse.bass as bass
import concourse.mybir as mybir
from concourse.bass_utils import run_bass_kernel_spmd

F32 = mybir.dt.float32
BF16 = mybir.dt.bfloat16
I32 = mybir.dt.int32
AF = mybir.ActivationFunctionType
ALU = mybir.AluOpType

D = 1024
SEQ = 2048
DEPTH = 4
NCORES = 8
E = 32
DFF = 256
ALPHA = (2.0 * DEPTH) ** 0.25
LN_EPS = 1e-5
UNIT = 1024
TM = 256
TE = 512
WIN_FM = 2176
WIN_ALL = 2688
PI = math.pi

C_DECAY = 0
C_XI = 512
C_ZETA = 768
C_BD = 896
C_CAUS = 1152
C_ID = 1280
C_HM = 1408
C_CD = 1412
C_INVF = 1413
C_SGN = 1414
C_ONES = 1416
NCONST = 1544

V_BADA = 0
V_LN1G = 192
V_LN1B = 224
V_LN2G = 256
V_LN2B = 288
V_BGATE = 320
V_CONVW = 416
NVEC = 512


def _consts():
    c = np.zeros((128, NCONST), np.float64)
    gam = 1.0 - 2.0 ** (-5.0 - np.arange(4))
    lg = np.log(gam)
    idx = np.arange(128)
    sc = 32 ** -0.5
    for h in range(4):
        diff = idx[None, :] - idx[:, None]
        dm = np.where(diff >= 0, np.exp(lg[h] * np.maximum(diff, 0)), 0.0) * sc
        c[:, C_DECAY + h * 128:C_DECAY + (h + 1) * 128] = dm
    hd = idx // 32
    xi = np.exp(lg[hd][:, None] * (idx[None, :] + 1.0))
    c[:, C_XI:C_XI + 128] = xi
    c[:, C_XI + 128:C_XI + 256] = xi
    zeta = np.exp(lg[hd][None, :] * (127.0 - idx)[:, None]) * sc
    c[:, C_ZETA:C_ZETA + 128] = zeta
    hv = np.arange(256) // 64
    c[:, C_BD:C_BD + 256] = (hd[:, None] == hv[None, :]).astype(np.float64)
    c[:, C_CAUS:C_CAUS + 128] = (idx[:, None] <= idx[None, :]).astype(np.float64)
    c[:, C_ID:C_ID + 128] = np.eye(128)
    for h in range(4):
        c[:, C_HM + h] = (hd == h)
    c[:, C_CD] = np.exp(lg[hd] * 128.0)
    j = idx % 32
    invf = 10000.0 ** (-(2.0 * (j % 16)) / 32.0)
    c[:, C_INVF] = invf
    c[:, C_SGN] = np.where(j < 16, -1.0, 1.0)
    c[:, C_ONES:C_ONES + 128] = 1.0
    return c.astype(np.float32)


def _win_perm():
    o_q, o_k, o_v, o_g, o_gu, o_gv, o_cb, o_cc, o_cx, o_code = 0, 128, 256, 512, 768, 1024, 1280, 1536, 1792, 2048
    r = np.arange(128)
    j = r % 32
    perm = (r // 32) * 32 + (j + 16) % 32
    cols = np.concatenate([
        o_q + r, o_q + perm, o_k + r, o_k + perm,
        o_g + np.arange(256), o_gu + np.arange(256),
        o_cb + np.arange(256), o_cc + np.arange(256), o_cx + np.arange(256),
        o_code + np.arange(384),
        o_v + np.arange(256), o_gv + np.arange(256)])
    assert cols.shape[0] == WIN_ALL
    return cols


class Prog:
    def __init__(self, nc, es):
        self.nc = nc
        self.es = es
        self.lw = {}
        self.rd = {}
        self.engs = {}
        for name, eng in (("pe", nc.tensor), ("act", nc.scalar), ("dve", nc.vector), ("pool", nc.gpsimd), ("sp", nc.sync)):
            sem = es.enter_context(nc.semaphore("sem_" + name))
            self.engs[name] = dict(eng=eng, sem=sem, cnt=0, waited={}, name=name)
        self.dsems = {}
        self.nwait = 0
        self.ninst = 0

    def dsem(self, name):
        if name not in self.dsems:
            self.dsems[name] = dict(sem=self.es.enter_context(self.nc.semaphore("d_" + name)), tot=0)
        return self.dsems[name]

    def _deps(self, en, reads, writes):
        e = self.engs[en]
        deps = []
        for k in reads:
            t = self.lw.get(k)
            if t is not None:
                deps.append(t)
        for k in writes:
            t = self.lw.get(k)
            if t is not None:
                deps.append(t)
            for t in self.rd.get(k, {}).values():
                deps.append(t)
        for (sem, val, owner) in deps:
            if en == "pe" and owner == "pe":
                continue
            sid = id(sem)
            if e["waited"].get(sid, 0) >= val:
                continue
            e["eng"].wait_ge(sem, val)
            e["waited"][sid] = val
            self.nwait += 1

    def _mark(self, tok, en, reads, writes):
        for k in reads:
            self.rd.setdefault(k, {})[en] = tok
        for k in writes:
            self.lw[k] = tok
            self.rd[k] = {}

    def op(self, en, fn, reads=(), writes=()):
        e = self.engs[en]
        self._deps(en, reads, writes)
        inst = fn(e["eng"])
        e["cnt"] += 1
        inst.then_inc(e["sem"], 1)
        self.ninst += 1
        self._mark((e["sem"], e["cnt"], en), en, reads, writes)

    def dma(self, q, semname, items, reads=(), writes=()):
        e = self.engs[q]
        ds = self.dsem(semname)
        self._deps(q, reads, writes)
        if ds["tot"] > 0 and e["waited"].get(id(ds["sem"]), 0) < ds["tot"]:
            e["eng"].wait_ge(ds["sem"], ds["tot"])
            e["waited"][id(ds["sem"])] = ds["tot"]
        for (o, i) in items:
            inst = e["eng"].dma_start(out=o, in_=i)
            ds["tot"] += 16
            inst.then_inc(ds["sem"], 16)
            self.ninst += 1
        self._mark((ds["sem"], ds["tot"], "dma_" + semname), "dma_" + semname + q, reads, writes)

    def barrier(self):
        toks = [(e["sem"], e["cnt"]) for e in self.engs.values() if e["cnt"] > 0]
        toks += [(d["sem"], d["tot"]) for d in self.dsems.values() if d["tot"] > 0]
        for e in self.engs.values():
            for (sem, val) in toks:
                if sem is e["sem"]:
                    continue
                sid = id(sem)
                if e["waited"].get(sid, 0) >= val:
                    continue
                e["eng"].wait_ge(sem, val)
                e["waited"][sid] = val

    def finish(self, q):
        e = self.engs[q]
        for d in self.dsems.values():
            if d["tot"] > 0:
                e["eng"].wait_ge(d["sem"], d["tot"])
        for o in self.engs.values():
            if o is not e and o["cnt"] > 0:
                e["eng"].wait_ge(o["sem"], o["cnt"])


def build(NB=4, NL=DEPTH, dbg=False):
    nc = bass.Bass("TRN2", target_bir_lowering=False)
    es = contextlib.ExitStack()
    P = Prog(nc, es)

    def din(name, shape, dt=F32):
        return nc.dram_tensor(name, list(shape), dt, kind="ExternalInput").ap()

    x_d = din("x", [NB, SEQ, D])
    cT_d = din("cT", [128, 8, NB])
    pos_d = din("pos", [NB, SEQ], I32)
    wada_d = din("w_ada", [DEPTH, D, 6 * D])
    win_d = din("w_in", [DEPTH, D, WIN_ALL])
    vecs_d = din("vecs", [NVEC, 128])
    const_d = din("consts", [128, NCONST])
    lng_d = din("gmlp_ln_g", [DEPTH, 256])
    lnb_d = din("gmlp_ln_b", [DEPTH, 256])
    wsp_d = din("w_spatial", [DEPTH, 4, 128, 128])
    bsp_d = din("b_spatial", [DEPTH, 4, 128])
    wbr_d = din("w_branch", [DEPTH, 768, D])
    wg_d = din("w_gate_up", [DEPTH, 384, D])
    wout_d = din("w_out", [DEPTH, D, D])
    wr_d = din("w_router", [DEPTH, D, E])
    br_d = din("b_router", [DEPTH, E])
    wgu_d = din("w_gu", [DEPTH, E, D, 2 * DFF])
    bgu_d = din("b_gu", [DEPTH, E, 2 * DFF])
    wd_d = din("w_down", [DEPTH, E, DFF, D])
    bd_d = din("b_down", [DEPTH, E, D])
    out_d = nc.dram_tensor("out", [NB, SEQ, D], F32, kind="ExternalOutput").ap()

    def sb(name, shape, dt=F32):
        return es.enter_context(nc.sbuf_tensor(name, list(shape), dt))

    XT = sb("XT", [128, 8, UNIT])
    h2T = sb("h2T", [128, 8, UNIT], BF16)
    CT = sb("CT", [128, NCONST])
    identb = sb("identb", [128, 128], BF16)
    vecT = sb("vecT", [128, NVEC])
    adaT = sb("adaT", [128, NL, 48, NB])
    dsc = sb("dsc", [128, NL, NB, 4, 8])
    cosT = sb("cosT", [128, UNIT])
    sinT = sb("sinT", [128, UNIT])
    wgt = sb("wgt", [128, UNIT // 128, E])
    state = sb("state", [128, NL, 256])
    state_bf = sb("state_bf", [128, NL, 256], BF16)
    zhalo = sb("zhalo", [128, NL, 2, 2])
    lngb = sb("lngb", [128, 2, 256])
    WmT = sb("WmT", [128, 4, 128], BF16)
    bsp_row = sb("bsp_row", [1, 512], BF16)
    brt_row = sb("brt_row", [1, E], BF16)
    Wr = sb("Wr", [128, 8, E], BF16)
    bdn = sb("bdn", [E, D], BF16)
    ones_bf = sb("ones_bf", [1, 128], BF16)
    small = sb("small", [128, 64])
    Win = sb("Win", [128, 8, WIN_ALL], BF16)
    Wbr = sb("Wbr", [128, 6, D], BF16)
    Wg = sb("Wg", [128, 3, D], BF16)
    Wout = sb("Wout", [128, 8, D], BF16)
    ARENA = 46 * 1024 // 4
    arena = sb("arena", [128, ARENA])
    apos = [0]

    def carve(n_elems, dt=F32):
        nb = n_elems * (4 if dt in (F32, I32) else 2)
        nw = (nb + 3) // 4
        a = apos[0]
        apos[0] += nw
        assert apos[0] <= ARENA, ("arena overflow", apos[0], ARENA)
        v = arena[:, a:a + nw]
        return v if dt == F32 else v.bitcast(dt)

    apos[0] = 0
    hT = carve(8 * TM, BF16).rearrange("p (k t) -> p k t", k=8)
    rskT = carve(6 * TM, BF16).rearrange("p (k t) -> p k t", k=6)
    codeT = carve(3 * TM, BF16).rearrange("p (k t) -> p k t", k=3)
    qr = carve(TM, BF16)
    kr = carve(TM, BF16)
    qxi = carve(TM, BF16)
    kmask = carve(4 * TM, BF16).rearrange("p (k t) -> p k t", k=4)
    f_a = carve(2 * TM)
    f_b = carve(2 * TM)
    f_c = carve(2 * TM)
    f_d = carve(2 * TM)
    f_e = carve(2 * TM)
    gsil = carve(2 * TM).rearrange("p (k t) -> p k t", k=2)
    uT = carve(2 * TM).rearrange("p (k t) -> p k t", k=2)
    zbuf = carve(2 * (TM + 2)).rearrange("p (k t) -> p k t", k=2)
    vtok = carve(256, BF16)
    vln = carve(256, BF16)
    Sd = carve(512, BF16)
    kz = carve(128, BF16)
    t_a = carve(256)
    t_b = carve(256)
    t_c = carve(256)
    xstage = carve(1024)
    mix_end = apos[0]
    apos[0] = 0
    Wgu = [carve(8 * 512, BF16).rearrange("p (k f) -> p k f", k=8) for _ in range(2)]
    Wd = [carve(2 * D, BF16).rearrange("p (k f) -> p k f", k=2) for _ in range(2)]
    bgu = [carve(512, BF16) for _ in range(2)]
    NCH = 2
    cg = [carve(256) for _ in range(NCH)]
    csig = [carve(256) for _ in range(NCH)]
    cu = [carve(256) for _ in range(NCH)]
    cp_ = [carve(256) for _ in range(NCH)]
    cact = [carve(256, BF16) for _ in range(4)]
    actT = [carve(2 * TE, BF16).rearrange("p (k t) -> p k t", k=2) for _ in range(2)]
    wgtT = carve(TE)
    rt_a = carve(E)
    rt_b = carve(E)
    top8 = carve(8)
    moe_end = apos[0]

    banks = [es.enter_context(nc.psum_tensor("ps%d" % i, [128, 512], F32)) for i in range(8)]
    bk = [0]

    def psn():
        i = bk[0] % 8
        bk[0] += 1
        return banks[i], "ps%d" % i

    op, dma = P.op, P.dma
    ident = CT[:, C_ID:C_ID + 128]
    ones_f = CT[:, C_ONES:C_ONES + 128]

    dma("sp", "const", [(CT[:], const_d)], writes=["CT"])
    op("dve", lambda v: v.tensor_copy(out=identb[:], in_=ident), reads=["CT"], writes=["identb"])
    op("dve", lambda v: v.memset(ones_bf[:], 1.0), writes=["ones_bf"])
    for r in range(NVEC // 128):
        dma("sp", "setup", [(f_a[:, 0:128], vecs_d[r * 128:(r + 1) * 128, :])], writes=["f_a"])
        pb, pk = psn()
        op("pe", lambda t: t.transpose(out=pb[:, 0:128], in_=f_a[:, 0:128], identity=ident), reads=["f_a", "CT"], writes=[pk])
        op("dve", lambda v: v.tensor_copy(out=vecT[:, r * 128:(r + 1) * 128], in_=pb[:, 0:128]), reads=[pk], writes=["vecT"])
    cact_t = t_a[:, 0:8 * NB].rearrange("p (k b) -> p k b", k=8)
    csg = t_b[:, 0:8 * NB].rearrange("p (k b) -> p k b", k=8)
    cbf = vtok[:, 0:8 * NB].rearrange("p (k b) -> p k b", k=8)
    dma("sp", "setup", [(cact_t, cT_d)], writes=["t_a"])
    op("act", lambda a: a.activation(out=csg, in_=cact_t, func=AF.Sigmoid), reads=["t_a"], writes=["t_b"])
    op("dve", lambda v: v.tensor_tensor(out=cbf, in0=cact_t, in1=csg, op=ALU.mult), reads=["t_a", "t_b"], writes=["vtok"])
    WA = Win[:, :, 0:1536]
    for l in range(NL):
        for pc in range(4):
            dma("pool", "wada", [(WA, wada_d[l, :, pc * 1536:(pc + 1) * 1536].rearrange("(k p) n -> p k n", p=128))], writes=["Win"])
            for jj in range(12):
                j = pc * 12 + jj
                pb, pk = psn()
                for kc in range(8):
                    op("pe", lambda t: t.matmul(pb[:, 0:NB], lhsT=WA[:, kc, jj * 128:(jj + 1) * 128], rhs=cbf[:, kc, :],
                                                start=(kc == 0), stop=(kc == 7)), reads=["Win", "vtok"], writes=[pk])
                op("dve", lambda v: v.tensor_scalar(out=adaT[:, l, j, :], in0=pb[:, 0:NB], scalar1=vecT[:, V_BADA + l * 48 + j:V_BADA + l * 48 + j + 1],
                                                    scalar2=None, op0=ALU.add), reads=[pk, "vecT"], writes=["adaT"])
    for l in range(NL):
        for b in range(NB):
            op("dve", lambda v: v.tensor_scalar(out=dsc[:, l, b, 0, :], in0=adaT[:, l, 8:16, b], scalar1=1.0, scalar2=None, op0=ALU.add), reads=["adaT"], writes=["dsc"])
            op("dve", lambda v: v.tensor_scalar(out=dsc[:, l, b, 1, :], in0=adaT[:, l, 16:24, b], scalar1=1.0, scalar2=1.0 / ALPHA, op0=ALU.add, op1=ALU.mult), reads=["adaT"], writes=["dsc"])
            op("dve", lambda v: v.tensor_scalar(out=dsc[:, l, b, 2, :], in0=adaT[:, l, 32:40, b], scalar1=1.0, scalar2=None, op0=ALU.add), reads=["adaT"], writes=["dsc"])
            op("dve", lambda v: v.tensor_scalar(out=dsc[:, l, b, 3, :], in0=adaT[:, l, 40:48, b], scalar1=1.0, scalar2=1.0 / ALPHA, op0=ALU.add, op1=ALU.mult), reads=["adaT"], writes=["dsc"])

    def col(ap2d, j):
        return ap2d[:, j:j + 1]

    def gelu_tanh(src_ps, srck, dst, dstk, n, tmp1, tmp1k, tmp2, tmp2k):
        op("act", lambda a: a.activation(out=tmp1[:, 0:n], in_=src_ps, func=AF.Square), reads=[srck], writes=[tmp1k])
        op("dve", lambda v: v.tensor_scalar(out=tmp1[:, 0:n], in0=tmp1[:, 0:n], scalar1=0.044715, scalar2=1.0, op0=ALU.mult, op1=ALU.add), reads=[tmp1k], writes=[tmp1k])
        op("dve", lambda v: v.tensor_tensor(out=tmp1[:, 0:n], in0=src_ps, in1=tmp1[:, 0:n], op=ALU.mult), reads=[srck, tmp1k], writes=[tmp1k])
        op("act", lambda a: a.activation(out=tmp2[:, 0:n], in_=tmp1[:, 0:n], func=AF.Sigmoid, scale=1.5957691216057308), reads=[tmp1k], writes=[tmp2k])
        op("dve", lambda v: v.tensor_tensor(out=dst, in0=src_ps, in1=tmp2[:, 0:n], op=ALU.mult), reads=[srck, tmp2k], writes=[dstk])

    def rstd_from_var(var_ap, out_ap, eps, rk, wk):
        op("dve", lambda v: v.tensor_scalar(out=out_ap, in0=var_ap, scalar1=float(eps), scalar2=None, op0=ALU.add), reads=rk, writes=wk)
        op("act", lambda a: a.activation(out=out_ap, in_=out_ap, func=AF.Sqrt), reads=wk, writes=wk)
        op("dve", lambda v: v.reciprocal(out=out_ap, in_=out_ap), reads=wk, writes=wk)

    for b in range(NB):
        for hf in range(SEQ // UNIT):
            tok0 = hf * UNIT
            P.barrier()
            posi = xstage.bitcast(I32)
            dma("sp", "setup", [(posi, pos_d[b:b + 1, tok0:tok0 + UNIT].partition_broadcast(128))], writes=["xstage"])
            tmpI = hT.rearrange("p k t -> p (k t)").bitcast(I32)
            op("dve", lambda v: v.tensor_copy(out=sinT[:], in_=posi), reads=["xstage"], writes=["sinT"])
            op("dve", lambda v: v.tensor_scalar(out=sinT[:], in0=sinT[:], scalar1=CT[:, C_INVF:C_INVF + 1], scalar2=None, op0=ALU.mult), reads=["sinT", "CT"], writes=["sinT"])
            op("dve", lambda v: v.tensor_scalar(out=cosT[:], in0=sinT[:], scalar1=PI / 2, scalar2=None, op0=ALU.add), reads=["sinT"], writes=["cosT"])
            for (tab, tk, sgn) in ((sinT, "sinT", True), (cosT, "cosT", False)):
                ki = tmpI
                kf = xstage
                op("dve", lambda v: v.tensor_scalar(out=ki, in0=tab[:], scalar1=1.0 / (2 * PI), scalar2=None, op0=ALU.mult), reads=[tk], writes=["hT"])
                op("dve", lambda v: v.tensor_copy(out=kf, in_=ki), reads=["hT", "sinT"], writes=["xstage"])
                op("dve", lambda v: v.scalar_tensor_tensor(out=tab[:], in0=kf, scalar=-2 * PI, in1=tab[:], op0=ALU.mult, op1=ALU.add), reads=["xstage", tk], writes=[tk])
                op("dve", lambda v: v.tensor_scalar(out=kf, in0=tab[:], scalar1=PI, scalar2=2 * PI, op0=ALU.is_gt, op1=ALU.mult), reads=[tk], writes=["xstage"])
                op("dve", lambda v: v.tensor_tensor(out=tab[:], in0=tab[:], in1=kf, op=ALU.subtract), reads=[tk, "xstage"], writes=[tk])
                op("dve", lambda v: v.tensor_scalar(out=kf, in0=tab[:], scalar1=-PI, scalar2=2 * PI, op0=ALU.is_lt, op1=ALU.mult), reads=[tk], writes=["xstage"])
                op("dve", lambda v: v.tensor_tensor(out=tab[:], in0=tab[:], in1=kf, op=ALU.add), reads=[tk, "xstage"], writes=[tk])
                op("dve", lambda v: v.tensor_scalar(out=tab[:], in0=tab[:], scalar1=-PI, scalar2=PI, op0=ALU.max, op1=ALU.min), reads=[tk], writes=[tk])
                op("act", lambda a: a.activation(out=tab[:], in_=tab[:], func=AF.Sin), reads=[tk], writes=[tk])
                if sgn:
                    op("dve", lambda v: v.tensor_scalar(out=tab[:], in0=tab[:], scalar1=CT[:, C_SGN:C_SGN + 1], scalar2=None, op0=ALU.mult), reads=[tk, "CT"], writes=[tk])
            for c in range(UNIT // 128):
                dma("sp", "xin", [(xstage, x_d[b, tok0 + c * 128:tok0 + (c + 1) * 128, :])], writes=["xstage"])
                for g4 in range(2):
                    pb, pk = psn()
                    for q in range(4):
                        kc = g4 * 4 + q
                        op("pe", lambda t: t.transpose(out=pb[:, q * 128:(q + 1) * 128], in_=xstage[:, kc * 128:(kc + 1) * 128], identity=ident),
                           reads=["xstage", "CT"], writes=[pk])
                    op("act", lambda a: a.activation(out=XT[:, g4 * 4:(g4 + 1) * 4, c * 128:(c + 1) * 128], in_=pb[:].rearrange("p (q t) -> p q t", q=4), func=AF.Identity),
                       reads=[pk], writes=["XT"])
            if hf == 0:
                op("dve", lambda v: v.memset(state[:], 0.0), writes=["state"])
                op("dve", lambda v: v.memset(state_bf[:], 0.0), writes=["state_bf"])
                op("dve", lambda v: v.memset(zhalo[:], 0.0), writes=["zhalo"])

            for l in range(NL):
                P.barrier()
                wl = lambda ap: ap.rearrange("(k p) n -> p k n", p=128)
                dma("pool", "wmix", [(Win[:, :, 0:1344], wl(win_d[l, :, 0:1344])), (Win[:, :, 1344:2688], wl(win_d[l, :, 1344:2688])), (Wbr[:], wl(wbr_d[l])), (Wg[:], wl(wg_d[l])), (Wout[:], wl(wout_d[l]))],
                    writes=["Win", "Wbr", "Wg", "Wout"])
                if True:
                    dma("sp", "setup", [(lngb[:, 0, :], lng_d[l:l + 1, :].partition_broadcast(128)), (lngb[:, 1, :], lnb_d[l:l + 1, :].partition_broadcast(128))], writes=["lngb"])
                    dma("pool", "wsm", [(bsp_row[:], bsp_d[l:l + 1].rearrange("o g c -> o (g c)")), (brt_row[:], br_d[l:l + 1, :]),
                                        (Wr[:], wr_d[l].rearrange("(k p) n -> p k n", p=128)), (bdn[:], bd_d[l])],
                        writes=["bsp_row", "brt_row", "Wr", "bdn"])
                    for g in range(4):
                        dma("sp", "setup", [(t_a[:, 0:128], wsp_d[l, g])], writes=["t_a"])
                        pb, pk = psn()
                        op("pe", lambda t: t.transpose(out=pb[:, 0:128], in_=t_a[:, 0:128], identity=ident), reads=["t_a", "CT"], writes=[pk])
                        op("dve", lambda v: v.tensor_tensor(out=WmT[:, g, :], in0=pb[:, 0:128], in1=CT[:, C_CAUS:C_CAUS + 128], op=ALU.mult), reads=[pk, "CT"], writes=["WmT"])
                A1 = dsc[:, l, b, 0, :]
                G1 = dsc[:, l, b, 1, :]
                A2 = dsc[:, l, b, 2, :]
                G2 = dsc[:, l, b, 3, :]
                for tj in range(UNIT // TM):
                    ts = slice(tj * TM, (tj + 1) * TM)
                    for kc in range(8):
                        op("act", lambda a: a.activation(out=hT[:, kc, :], in_=XT[:, kc, ts], func=AF.Identity, scale=col(A1, kc), bias=adaT[:, l, kc, b:b + 1]),
                           reads=["XT", "dsc", "adaT"], writes=["hT"])

                    def proj(c0, nch):
                        pb, pk = psn()
                        for q in range(nch):
                            for kc in range(8):
                                op("pe", lambda t: t.matmul(pb[:, q * TM:(q + 1) * TM], lhsT=Win[:, kc, (c0 + q) * 128:(c0 + q + 1) * 128], rhs=hT[:, kc, :],
                                                            start=(kc == 0), stop=(kc == 7)), reads=["Win", "hT"], writes=[pk])
                        return pb, pk
                    for (c0, dst, dk) in ((0, qr, "qr"), (2, kr, "kr")):
                        pb, pk = proj(c0, 2)
                        op("dve", lambda v: v.tensor_tensor(out=f_a[:, 0:TM], in0=pb[:, 0:TM], in1=cosT[:, ts], op=ALU.mult), reads=[pk, "cosT"], writes=["f_a"])
                        op("dve", lambda v: v.tensor_tensor(out=f_b[:, 0:TM], in0=pb[:, TM:2 * TM], in1=sinT[:, ts], op=ALU.mult), reads=[pk, "sinT"], writes=["f_b"])
                        op("dve", lambda v: v.tensor_tensor(out=dst, in0=f_a[:, 0:TM], in1=f_b[:, 0:TM], op=ALU.add), reads=["f_a", "f_b"], writes=[dk])
                    op("dve", lambda v: v.tensor_tensor(out=qxi, in0=qr, in1=CT[:, C_XI:C_XI + TM], op=ALU.mult), reads=["qr", "CT"], writes=["qxi"])
                    for h in range(4):
                        op("act", lambda a: a.activation(out=kmask[:, h, :], in_=kr, func=AF.Identity, scale=CT[:, C_HM + h:C_HM + h + 1]), reads=["kr", "CT"], writes=["kmask"])
                    pb, pk = proj(4, 2)
                    op("act", lambda a: a.activation(out=f_a, in_=pb[:], func=AF.Sigmoid), reads=[pk], writes=["f_a"])
                    op("dve", lambda v: v.tensor_tensor(out=gsil.rearrange("p k t -> p (k t)"), in0=pb[:], in1=f_a, op=ALU.mult), reads=[pk, "f_a"], writes=["gsil"])
                    pb, pk = proj(6, 2)
                    gelu_tanh(pb[:], pk, uT.rearrange("p k t -> p (k t)"), "uT", 2 * TM, f_b, "f_b", f_c, "f_c")
                    pcb, kcb = proj(8, 2)
                    pcc, kcc = proj(10, 2)
                    pcx, kcx = proj(12, 2)
                    op("act", lambda a: a.activation(out=f_a, in_=pcc[:], func=AF.Identity), reads=[kcc], writes=["f_a"])
                    for ch in range(2):
                        op("dve", lambda v: v.tensor_copy(out=zbuf[:, ch, 0:2], in_=zhalo[:, l, ch, :]), reads=["zhalo"], writes=["zbuf"])
                        op("dve", lambda v: v.tensor_tensor(out=zbuf[:, ch, 2:TM + 2], in0=pcx[:, ch * TM:(ch + 1) * TM], in1=f_a[:, ch * TM:(ch + 1) * TM], op=ALU.mult),
                           reads=[kcx, "f_a"], writes=["zbuf"])
                        op("dve", lambda v: v.tensor_copy(out=zhalo[:, l, ch, :], in_=zbuf[:, ch, TM:TM + 2]), reads=["zbuf"], writes=["zhalo"])
                        w0 = vecT[:, V_CONVW + (l * 3 + 0) * 2 + ch:V_CONVW + (l * 3 + 0) * 2 + ch + 1]
                        w1 = vecT[:, V_CONVW + (l * 3 + 1) * 2 + ch:V_CONVW + (l * 3 + 1) * 2 + ch + 1]
                        w2 = vecT[:, V_CONVW + (l * 3 + 2) * 2 + ch:V_CONVW + (l * 3 + 2) * 2 + ch + 1]
                        acc = f_d[:, ch * TM:(ch + 1) * TM]
                        op("dve", lambda v: v.tensor_scalar(out=acc, in0=zbuf[:, ch, 2:TM + 2], scalar1=w2, scalar2=None, op0=ALU.mult), reads=["zbuf", "vecT"], writes=["f_d"])
                        op("dve", lambda v: v.scalar_tensor_tensor(out=acc, in0=zbuf[:, ch, 1:TM + 1], scalar=w1, in1=acc, op0=ALU.mult, op1=ALU.add), reads=["zbuf", "vecT", "f_d"], writes=["f_d"])
                        op("dve", lambda v: v.scalar_tensor_tensor(out=acc, in0=zbuf[:, ch, 0:TM], scalar=w0, in1=acc, op0=ALU.mult, op1=ALU.add), reads=["zbuf", "vecT", "f_d"], writes=["f_d"])
                    op("dve", lambda v: v.tensor_tensor(out=rskT[:, 4:6, :].rearrange("p k t -> p (k t)"), in0=pcb[:], in1=f_d, op=ALU.mult), reads=[kcb, "f_d"], writes=["rskT"])
                    pb, pk = proj(14, 2)
                    op("act", lambda a: a.activation(out=codeT[:, 0:2, :].rearrange("p k t -> p (k t)"), in_=pb[:], func=AF.Identity), reads=[pk], writes=["codeT"])
                    pb, pk = proj(16, 1)
                    op("act", lambda a: a.activation(out=codeT[:, 2, :], in_=pb[:, 0:TM], func=AF.Identity), reads=[pk], writes=["codeT"])
                    for cc_ in range(TM // 128):
                        cs = slice(cc_ * 128, (cc_ + 1) * 128)
                        ptm, ktm = psn()
                        for kc in range(8):
                            op("pe", lambda t: t.matmul(ptm[:], lhsT=hT[:, kc, cs], rhs=Win[:, kc, WIN_FM:WIN_ALL], start=(kc == 0), stop=(kc == 7)), reads=["hT", "Win"], writes=[ktm])
                        op("act", lambda a: a.activation(out=vtok, in_=ptm[:, 0:256], func=AF.Identity), reads=[ktm], writes=["vtok"])
                        gelu_tanh(ptm[:, 256:512], ktm, t_a, "t_a", 256, t_b, "t_b", t_c, "t_c")
                        op("dve", lambda v: v.bn_stats(out=small[:, 0:6], in_=t_a), reads=["t_a"], writes=["small"])
                        op("dve", lambda v: v.bn_aggr(out=small[:, 8:10], in_=small[:, 0:6]), reads=["small"], writes=["small"])
                        rstd_from_var(small[:, 9:10], small[:, 10:11], LN_EPS, ["small"], ["small"])
                        op("dve", lambda v: v.tensor_scalar(out=t_a, in0=t_a, scalar1=small[:, 8:9], scalar2=small[:, 10:11], op0=ALU.subtract, op1=ALU.mult), reads=["t_a", "small"], writes=["t_a"])
                        op("dve", lambda v: v.tensor_tensor(out=t_a, in0=t_a, in1=lngb[:, 0, :], op=ALU.mult), reads=["t_a", "lngb"], writes=["t_a"])
                        op("dve", lambda v: v.tensor_tensor(out=vln, in0=t_a, in1=lngb[:, 1, :], op=ALU.add), reads=["t_a", "lngb"], writes=["vln"])
                        pz, kzk = psn()
                        for g in range(4):
                            op("pe", lambda t: t.matmul(pz[:, g * 64:(g + 1) * 64], lhsT=WmT[:, g, :], rhs=vln[:, g * 64:(g + 1) * 64], start=True, stop=False), reads=["WmT", "vln"], writes=[kzk])
                            op("pe", lambda t: t.matmul(pz[:, g * 64:(g + 1) * 64], lhsT=bsp_row[0:1, g * 128:(g + 1) * 128], rhs=ones_bf[0:1, 0:64], start=False, stop=True),
                               reads=["bsp_row", "ones_bf"], writes=[kzk])
                        op("act", lambda a: a.activation(out=t_b, in_=pz[:, 0:256], func=AF.Identity), reads=[kzk], writes=["t_b"])
                        ptr, ktr = psn()
                        for q in range(2):
                            op("pe", lambda t: t.transpose(out=ptr[:, q * 128:(q + 1) * 128], in_=t_b[:, q * 128:(q + 1) * 128], identity=ident), reads=["t_b", "CT"], writes=[ktr])
                        op("dve", lambda v: v.tensor_tensor(out=rskT[:, 2:4, cs], in0=ptr[:, 0:256].rearrange("p (q t) -> p q t", q=2), in1=uT[:, :, cs], op=ALU.mult), reads=[ktr, "uT"], writes=["rskT"])
                        psc, ksc = psn()
                        for h in range(4):
                            op("pe", lambda t: t.matmul(psc[:, h * 128:(h + 1) * 128], lhsT=kmask[:, h, cs], rhs=qr[:, cs], start=True, stop=True), reads=["kmask", "qr"], writes=[ksc])
                        op("dve", lambda v: v.tensor_tensor(out=Sd, in0=psc[:], in1=CT[:, C_DECAY:C_DECAY + 512], op=ALU.mult), reads=[ksc, "CT"], writes=["Sd"])
                        po, ko = psn()
                        op("pe", lambda t: t.matmul(po[:, 0:256], lhsT=qxi[:, cs], rhs=state_bf[:, l, :], start=True, stop=False), reads=["qxi", "state_bf"], writes=[ko])
                        for h in range(4):
                            op("pe", lambda t: t.matmul(po[:, h * 64:(h + 1) * 64], lhsT=Sd[:, h * 128:(h + 1) * 128], rhs=vtok[:, h * 64:(h + 1) * 64], start=False, stop=(h == 3)),
                               reads=["Sd", "vtok"], writes=[ko])
                        for h in range(4):
                            op("dve", lambda v: v.bn_stats(out=small[:, 16 + h * 6:22 + h * 6], in_=po[:, h * 64:(h + 1) * 64]), reads=[ko], writes=["small"])
                        for h in range(4):
                            op("dve", lambda v: v.bn_aggr(out=small[:, 40 + 2 * h:42 + 2 * h], in_=small[:, 16 + h * 6:22 + h * 6]), reads=["small"], writes=["small"])
                        sm3 = small[:, 40:48].rearrange("p (h t) -> p h t", t=2)
                        rstd_from_var(sm3[:, :, 1], small[:, 48:52], LN_EPS, ["small"], ["small"])
                        for h in range(4):
                            op("dve", lambda v: v.tensor_scalar(out=t_c[:, h * 64:(h + 1) * 64], in0=po[:, h * 64:(h + 1) * 64], scalar1=small[:, 40 + 2 * h:41 + 2 * h],
                                                                scalar2=small[:, 48 + h:49 + h], op0=ALU.subtract, op1=ALU.mult), reads=[ko, "small"], writes=["t_c"])
                        ptr, ktr = psn()
                        for q in range(2):
                            op("pe", lambda t: t.transpose(out=ptr[:, q * 128:(q + 1) * 128], in_=t_c[:, q * 128:(q + 1) * 128], identity=ident), reads=["t_c", "CT"], writes=[ktr])
                        op("dve", lambda v: v.tensor_tensor(out=rskT[:, 0:2, cs], in0=ptr[:, 0:256].rearrange("p (q t) -> p q t", q=2), in1=gsil[:, :, cs], op=ALU.mult), reads=[ktr, "gsil"], writes=["rskT"])
                        pkt, kkt = psn()
                        pkt_b = pkt[:].bitcast(BF16)
                        op("pe", lambda t: t.transpose(out=pkt_b[:, 0:128], in_=kr[:, cs], identity=identb[:]), reads=["kr", "identb"], writes=[kkt])
                        op("dve", lambda v: v.tensor_tensor(out=kz, in0=pkt_b[:, 0:128], in1=CT[:, C_ZETA:C_ZETA + 128], op=ALU.mult), reads=[kkt, "CT"], writes=["kz"])
                        pkv, kkv = psn()
                        op("pe", lambda t: t.matmul(pkv[:, 0:256], lhsT=kz, rhs=vtok, start=True, stop=True), reads=["kz", "vtok"], writes=[kkv])
                        op("dve", lambda v: v.tensor_tensor(out=t_b, in0=pkv[:, 0:256], in1=CT[:, C_BD:C_BD + 256], op=ALU.mult), reads=[kkv, "CT"], writes=["t_b"])
                        op("dve", lambda v: v.scalar_tensor_tensor(out=state[:, l, :], in0=state[:, l, :], scalar=CT[:, C_CD:C_CD + 1], in1=t_b, op0=ALU.mult, op1=ALU.add),
                           reads=["state", "CT", "t_b"], writes=["state"])
                        op("act", lambda a: a.activation(out=state_bf[:, l, :], in_=state[:, l, :], func=AF.Identity), reads=["state"], writes=["state_bf"])
                    mergedT = hT
                    for dc in range(8):
                        dsl = slice(dc * 128, (dc + 1) * 128)
                        for i in range(3):
                            py, ky = psn()
                            for kk in range(2):
                                op("pe", lambda t: t.matmul(py[:, 0:TM], lhsT=Wbr[:, i * 2 + kk, dsl], rhs=rskT[:, i * 2 + kk, :], start=(kk == 0), stop=(kk == 1)), reads=["Wbr", "rskT"], writes=[ky])
                            op("pe", lambda t: t.matmul(py[:, TM:2 * TM], lhsT=Wg[:, i, dsl], rhs=codeT[:, i, :], start=True, stop=True), reads=["Wg", "codeT"], writes=[ky])
                            bg = vecT[:, V_BGATE + (l * 3 + i) * 8 + dc:V_BGATE + (l * 3 + i) * 8 + dc + 1]
                            op("act", lambda a: a.activation(out=f_a[:, 0:TM], in_=py[:, TM:2 * TM], func=AF.Sigmoid, bias=bg), reads=[ky, "vecT"], writes=["f_a"])
                            if i == 0:
                                op("dve", lambda v: v.tensor_tensor(out=f_b[:, 0:TM], in0=py[:, 0:TM], in1=f_a[:, 0:TM], op=ALU.mult), reads=[ky, "f_a"], writes=["f_b"])
                            else:
                                op("dve", lambda v: v.tensor_tensor(out=f_c[:, 0:TM], in0=py[:, 0:TM], in1=f_a[:, 0:TM], op=ALU.mult), reads=[ky, "f_a"], writes=["f_c"])
                                if i == 1:
                                    op("dve", lambda v: v.tensor_tensor(out=f_b[:, 0:TM], in0=f_b[:, 0:TM], in1=f_c[:, 0:TM], op=ALU.add), reads=["f_b", "f_c"], writes=["f_b"])
                                else:
                                    op("dve", lambda v: v.tensor_tensor(out=mergedT[:, dc, :], in0=f_b[:, 0:TM], in1=f_c[:, 0:TM], op=ALU.add), reads=["f_b", "f_c"], writes=["hT"])
                    for dc in range(8):
                        pm, km = psn()
                        for kc in range(8):
                            op("pe", lambda t: t.matmul(pm[:, 0:TM], lhsT=Wout[:, kc, dc * 128:(dc + 1) * 128], rhs=mergedT[:, kc, :], start=(kc == 0), stop=(kc == 7)), reads=["Wout", "hT"], writes=[km])
                        op("dve", lambda v: v.scalar_tensor_tensor(out=XT[:, dc, ts], in0=pm[:, 0:TM], scalar=col(G1, dc), in1=XT[:, dc, ts], op0=ALU.mult, op1=ALU.add),
                           reads=[km, "dsc", "XT"], writes=["XT"])
                    layer_norm_tile = None

                    def ln_tile(tsl, n, gvrow, bvrow, post):
                        ps1, k1 = psn()
                        for kc in range(8):
                            op("pe", lambda t: t.matmul(ps1[:, 0:n], lhsT=ones_f, rhs=XT[:, kc, tsl], start=(kc == 0), stop=(kc == 7)), reads=["CT", "XT"], writes=[k1])
                        ps2, k2 = psn()
                        for kc in range(8):
                            sq = (f_a, f_b)[kc % 2]
                            sqk = ("f_a", "f_b")[kc % 2]
                            op("act", lambda a: a.activation(out=sq[:, 0:n], in_=XT[:, kc, tsl], func=AF.Square), reads=["XT"], writes=[sqk])
                            op("pe", lambda t: t.matmul(ps2[:, 0:n], lhsT=ones_f, rhs=sq[:, 0:n], start=(kc == 0), stop=(kc == 7)), reads=["CT", sqk], writes=[k2])
                        mean = f_c[:, 0:n]
                        rstd = f_d[:, 0:n]
                        nmr = f_e[:, 0:n]
                        op("act", lambda a: a.activation(out=mean, in_=ps1[:, 0:n], func=AF.Identity, scale=1.0 / D), reads=[k1], writes=["f_c"])
                        op("dve", lambda v: v.tensor_tensor(out=nmr, in0=mean, in1=mean, op=ALU.mult), reads=["f_c"], writes=["f_e"])
                        op("dve", lambda v: v.scalar_tensor_tensor(out=rstd, in0=ps2[:, 0:n], scalar=1.0 / D, in1=nmr, op0=ALU.mult, op1=ALU.subtract), reads=[k2, "f_e"], writes=["f_d"])
                        rstd_from_var(rstd, rstd, LN_EPS / (ALPHA * ALPHA), ["f_d"], ["f_d"])
                        op("dve", lambda v: v.scalar_tensor_tensor(out=nmr, in0=mean, scalar=-1.0, in1=rstd, op0=ALU.mult, op1=ALU.mult), reads=["f_c", "f_d"], writes=["f_e"])
                        for kc in range(8):
                            xn = (f_a, f_b)[kc % 2][:, 0:n]
                            xk = ("f_a", "f_b")[kc % 2]
                            op("dve", lambda v: v.tensor_tensor(out=xn, in0=XT[:, kc, tsl], in1=rstd, op=ALU.mult), reads=["XT", "f_d"], writes=[xk])
                            op("dve", lambda v: v.tensor_tensor(out=xn, in0=xn, in1=nmr, op=ALU.add), reads=[xk, "f_e"], writes=[xk])
                            op("act", lambda a: a.activation(out=XT[:, kc, tsl], in_=xn, func=AF.Identity, scale=vecT[:, gvrow + kc:gvrow + kc + 1], bias=vecT[:, bvrow + kc:bvrow + kc + 1]),
                               reads=[xk, "vecT"], writes=["XT"])
                            if post is not None:
                                post(kc)
                    ln_tile(ts, TM, V_LN1G + l * 8, V_LN1B + l * 8,
                            lambda kc: op("act", lambda a: a.activation(out=h2T[:, kc, ts], in_=XT[:, kc, ts], func=AF.Identity, scale=col(A2, kc), bias=adaT[:, l, 24 + kc, b:b + 1]),
                                          reads=["XT", "dsc", "adaT"], writes=["h2T"]))

                P.barrier()
                NS = UNIT // 128
                for s in range(NS):
                    ss = slice(s * 128, (s + 1) * 128)
                    pr, kr_ = psn()
                    for kc in range(8):
                        op("pe", lambda t: t.matmul(pr[:, 0:E], lhsT=h2T[:, kc, ss], rhs=Wr[:, kc, :], start=(kc == 0), stop=False), reads=["h2T", "Wr"], writes=[kr_])
                    op("pe", lambda t: t.matmul(pr[:, 0:E], lhsT=ones_bf[0:1, :], rhs=brt_row[0:1, :], start=False, stop=True), reads=["ones_bf", "brt_row"], writes=[kr_])
                    op("dve", lambda v: v.tensor_copy(out=rt_a, in_=pr[:, 0:E]), reads=[kr_], writes=["rt_a"])
                    op("dve", lambda v: v.max(out=top8, in_=rt_a), reads=["rt_a"], writes=["top8"])
                    op("dve", lambda v: v.tensor_scalar(out=small[:, 56:57], in0=top8[:, 0:1], scalar1=-1.0, scalar2=None, op0=ALU.mult), reads=["top8"], writes=["small"])
                    op("act", lambda a: a.activation(out=rt_b, in_=rt_a, func=AF.Exp, bias=small[:, 56:57]), reads=["rt_a", "small"], writes=["rt_b"])
                    op("dve", lambda v: v.scalar_tensor_tensor(out=rt_b, in0=rt_a, scalar=top8[:, 3:4], in1=rt_b, op0=ALU.is_ge, op1=ALU.mult), reads=["rt_a", "top8", "rt_b"], writes=["rt_b"])
                    op("dve", lambda v: v.reduce_sum(out=small[:, 57:58], in_=rt_b, axis=mybir.AxisListType.X), reads=["rt_b"], writes=["small"])
                    op("dve", lambda v: v.reciprocal(out=small[:, 58:59], in_=small[:, 57:58]), reads=["small"], writes=["small"])
                    op("dve", lambda v: v.tensor_scalar(out=wgt[:, s, :], in0=rt_b, scalar1=small[:, 58:59], scalar2=None, op0=ALU.mult), reads=["rt_b", "small"], writes=["wgt"])
                for tt in range(UNIT // TE):
                    for s4 in range(4):
                        s = tt * 4 + s4
                        pw, kw = psn()
                        op("pe", lambda t: t.transpose(out=pw[0:E, 0:128], in_=wgt[:, s, :], identity=ident), reads=["wgt", "CT"], writes=[kw])
                        op("act", lambda a: a.activation(out=wgtT.bitcast(BF16)[0:E, s4 * 128:(s4 + 1) * 128], in_=pw[0:E, 0:128], func=AF.Identity), reads=[kw], writes=["wgtT"])
                    for dc in range(8):
                        pbd, kbd = psn()
                        op("pe", lambda t: t.matmul(pbd[:], lhsT=bdn[:, dc * 128:(dc + 1) * 128], rhs=wgtT.bitcast(BF16)[0:E, 0:TE], start=True, stop=True), reads=["bdn", "wgtT"], writes=[kbd])
                        xk = "XT%d_%d" % (tt, dc)
                        op("dve", lambda v: v.scalar_tensor_tensor(out=XT[:, dc, tt * TE:(tt + 1) * TE], in0=pbd[:], scalar=col(G2, dc), in1=XT[:, dc, tt * TE:(tt + 1) * TE], op0=ALU.mult, op1=ALU.add),
                           reads=[kbd, "dsc", "XT"], writes=["XT", xk])

                def load_gu(e):
                    sl = e % 2
                    dma("pool", "wexp%d" % sl,
                        [(Wgu[sl], wgu_d[l, e].rearrange("(k p) n -> p k n", p=128)),
                         (bgu[sl][0:1, :], bgu_d[l, e:e + 1, :])],
                        writes=["Wgu%d" % sl, "bgu%d" % sl])

                def load_d(e):
                    sl = e % 2
                    dma("pool", "wexd%d" % sl,
                        [(Wd[sl], wd_d[l, e].rearrange("(k p) n -> p k n", p=128))],
                        writes=["Wd%d" % sl])

                def tail(e, tt, ai):
                    sl = e % 2
                    for dc in range(8):
                        pyd, kyd = psn()
                        for fc in range(2):
                            op("pe", lambda t: t.matmul(pyd[:], lhsT=Wd[sl][:, fc, dc * 128:(dc + 1) * 128], rhs=actT[ai][:, fc, :], start=(fc == 0), stop=(fc == 1)),
                               reads=["Wd%d" % sl, "actT%d" % ai], writes=[kyd])
                        xk = "XT%d_%d" % (tt, dc)
                        op("dve", lambda v: v.scalar_tensor_tensor(out=XT[:, dc, tt * TE:(tt + 1) * TE], in0=pyd[:], scalar=col(G2, dc), in1=XT[:, dc, tt * TE:(tt + 1) * TE], op0=ALU.mult, op1=ALU.add),
                           reads=[kyd, "dsc", xk], writes=[xk])

                load_gu(0)
                load_d(0)
                ci = 0
                ui = 0
                pend = None
                for e in range(E):
                    sl = e % 2
                    if e + 1 < E:
                        load_gu(e + 1)
                    for tt in range(UNIT // TE):
                        ai = ui % 2
                        ui += 1
                        pT, kT = psn()
                        pT_b = pT[:].bitcast(BF16)
                        for s4 in range(4):
                            s = tt * 4 + s4
                            ss = slice(s * 128, (s + 1) * 128)
                            pg, kg = psn()
                            for kc in range(8):
                                op("pe", lambda t: t.matmul(pg[:], lhsT=h2T[:, kc, ss], rhs=Wgu[sl][:, kc, :], start=(kc == 0), stop=False), reads=["h2T", "Wgu%d" % sl], writes=[kg])
                            op("pe", lambda t: t.matmul(pg[:], lhsT=ones_bf[0:1, :], rhs=bgu[sl][0:1, :], start=False, stop=True), reads=["ones_bf", "bgu%d" % sl], writes=[kg])
                            c3 = ci % NCH
                            c4 = ci % 4
                            ci += 1
                            g_, sg_, u_, p_, a_ = cg[c3], csig[c3], cu[c3], cp_[c3], cact[c4]
                            op("dve", lambda v: v.tensor_scalar(out=g_, in0=pg[:, 0:256], scalar1=7.0, scalar2=None, op0=ALU.min), reads=[kg], writes=["cg%d" % c3])
                            op("act", lambda a: a.activation(out=sg_, in_=g_, func=AF.Sigmoid, scale=1.702), reads=["cg%d" % c3], writes=["cs%d" % c3])
                            op("dve", lambda v: v.tensor_scalar(out=u_, in0=pg[:, 256:512], scalar1=7.0, scalar2=-7.0, op0=ALU.min, op1=ALU.max), reads=[kg], writes=["cu%d" % c3])
                            op("dve", lambda v: v.tensor_scalar(out=u_, in0=u_, scalar1=1.0, scalar2=wgt[:, s, e:e + 1], op0=ALU.add, op1=ALU.mult), reads=["cu%d" % c3, "wgt"], writes=["cu%d" % c3])
                            op("dve", lambda v: v.tensor_tensor(out=p_, in0=g_, in1=u_, op=ALU.mult), reads=["cg%d" % c3, "cu%d" % c3], writes=["cp%d" % c3])
                            op("dve", lambda v: v.tensor_tensor(out=a_, in0=p_, in1=sg_, op=ALU.mult), reads=["cp%d" % c3, "cs%d" % c3], writes=["ca%d" % c4])
                            for fc in range(2):
                                op("pe", lambda t: t.transpose(out=pT_b[:, fc * TE + s4 * 128:fc * TE + (s4 + 1) * 128], in_=a_[:, fc * 128:(fc + 1) * 128], identity=identb[:]),
                                   reads=["ca%d" % c4, "identb"], writes=[kT])
                        op("act", lambda a: a.activation(out=actT[ai].rearrange("p k t -> p (k t)"), in_=pT_b[:, 0:2 * TE], func=AF.Identity), reads=[kT], writes=["actT%d" % ai])
                        if pend is not None:
                            tail(*pend)
                        if tt == 0 and e + 1 < E:
                            load_d(e + 1)
                        pend = (e, tt, ai)
                tail(*pend)
                pend = None
                for tt in range(UNIT // TE):
                    for dc in range(8):
                        pass
                P.barrier()
                for tq in range(UNIT // TM):
                    ln_tile(slice(tq * TM, (tq + 1) * TM), TM, V_LN2G + l * 8, V_LN2B + l * 8, None)

            P.barrier()
            for c in range(UNIT // 128):
                for g4 in range(2):
                    pb, pk = psn()
                    for q in range(4):
                        kc = g4 * 4 + q
                        op("pe", lambda t: t.transpose(out=pb[:, q * 128:(q + 1) * 128], in_=XT[:, kc, c * 128:(c + 1) * 128], identity=ident), reads=["XT", "CT"], writes=[pk])
                    op("act", lambda a: a.activation(out=xstage[:, g4 * 512:(g4 + 1) * 512], in_=pb[:], func=AF.Identity), reads=[pk], writes=["xstage"])
                dma("sp", "xout", [(out_d[b, tok0 + c * 128:tok0 + (c + 1) * 128, :], xstage)], reads=["xstage"])
    P.finish("sp")
    print("instructions", P.ninst, "waits", P.nwait, "arena mix/moe", mix_end * 4, moe_end * 4)
    es.close()
    return nc


_CONSTS = None


def _prep(inputs, NB, cores):
    global _CONSTS
    if _CONSTS is None:
        _CONSTS = _consts()
    g = lambda k: np.asarray(inputs[k])
    L = DEPTH
    perm = _win_perm()
    w_in = np.ascontiguousarray(g("w_in")[:, :, perm])
    vecs = np.zeros((NVEC, 128), np.float32)
    vecs[V_BADA:V_BADA + 192] = g("b_ada").reshape(L * 48, 128)
    vecs[V_LN1G:V_LN1G + 32] = g("ln1_g").reshape(L * 8, 128)
    vecs[V_LN1B:V_LN1B + 32] = g("ln1_b").reshape(L * 8, 128)
    vecs[V_LN2G:V_LN2G + 32] = g("ln2_g").reshape(L * 8, 128)
    vecs[V_LN2B:V_LN2B + 32] = g("ln2_b").reshape(L * 8, 128)
    vecs[V_BGATE:V_BGATE + 96] = g("b_gate").reshape(L * 3 * 8, 128)
    vecs[V_CONVW:V_CONVW + 24] = g("conv_w").reshape(L * 3 * 2, 128)
    shared = {
        "w_ada": g("w_ada"), "w_in": w_in, "vecs": vecs, "consts": _CONSTS,
        "gmlp_ln_g": g("gmlp_ln_g"), "gmlp_ln_b": g("gmlp_ln_b"),
        "w_spatial": g("w_spatial"), "b_spatial": g("b_spatial"),
        "w_branch": np.ascontiguousarray(g("w_branch").reshape(L, 768, D)),
        "w_gate_up": np.ascontiguousarray(g("w_gate_up").reshape(L, 384, D)),
        "w_out": g("w_out"), "w_router": g("w_router"), "b_router": g("b_router"),
        "w_gu": g("w_gu"), "b_gu": g("b_gu"), "w_down": g("w_down"), "b_down": g("b_down"),
    }
    x = g("x")
    c = g("c")
    pos = g("positions").astype(np.int32)
    maps = []
    for i in range(cores):
        bs = slice(i * NB, (i + 1) * NB)
        cT = np.ascontiguousarray(c[bs].reshape(NB, 8, 128).transpose(2, 1, 0))
        m = dict(shared)
        m.update({"x": np.ascontiguousarray(x[bs]), "cT": cT, "pos": np.ascontiguousarray(pos[bs])})
        maps.append(m)
    return maps


def kernel(**inputs):
    NB = 32 // NCORES
    nc = build(NB=NB, NL=DEPTH)
    maps = _prep(inputs, NB, NCORES)
    res = run_bass_kernel_spmd(nc, maps, core_ids=list(range(NCORES)))
    out = np.concatenate([np.asarray(r["out"]) for r in res.results], axis=0)
    return out.astype(np.float32)
```

```python
import contextlib
import math
import numpy as np
import concourse.bass as bass
import concourse.mybir as mybir
from concourse.bass_utils import run_bass_kernel_spmd

F32 = mybir.dt.float32
BF16 = mybir.dt.bfloat16
I32 = mybir.dt.int32
AF = mybir.ActivationFunctionType
ALU = mybir.AluOpType

D = 1024
SEQ = 2048
DEPTH = 4
NCORES = 8
E = 32
DFF = 256
ALPHA = (2.0 * DEPTH) ** 0.25
LN_EPS = 1e-5
UNIT = 1024
TM = 256
TE = 512
WIN_FM = 2176
WIN_ALL = 2688
PI = math.pi

C_DECAY = 0
C_XI = 512
C_ZETA = 768
C_BD = 896
C_CAUS = 1152
C_ID = 1280
C_HM = 1408
C_CD = 1412
C_INVF = 1413
C_SGN = 1414
C_ONES = 1416
NCONST = 1544

V_BADA = 0
V_LN1G = 192
V_LN1B = 224
V_LN2G = 256
V_LN2B = 288
V_BGATE = 320
V_CONVW = 416
NVEC = 512


def _consts():
    c = np.zeros((128, NCONST), np.float64)
    gam = 1.0 - 2.0 ** (-5.0 - np.arange(4))
    lg = np.log(gam)
    idx = np.arange(128)
    sc = 32 ** -0.5
    for h in range(4):
        diff = idx[None, :] - idx[:, None]
        dm = np.where(diff >= 0, np.exp(lg[h] * np.maximum(diff, 0)), 0.0) * sc
        c[:, C_DECAY + h * 128:C_DECAY + (h + 1) * 128] = dm
    hd = idx // 32
    xi = np.exp(lg[hd][:, None] * (idx[None, :] + 1.0))
    c[:, C_XI:C_XI + 128] = xi
    c[:, C_XI + 128:C_XI + 256] = xi
    zeta = np.exp(lg[hd][None, :] * (127.0 - idx)[:, None]) * sc
    c[:, C_ZETA:C_ZETA + 128] = zeta
    hv = np.arange(256) // 64
    c[:, C_BD:C_BD + 256] = (hd[:, None] == hv[None, :]).astype(np.float64)
    c[:, C_CAUS:C_CAUS + 128] = (idx[:, None] <= idx[None, :]).astype(np.float64)
    c[:, C_ID:C_ID + 128] = np.eye(128)
    for h in range(4):
        c[:, C_HM + h] = (hd == h)
    c[:, C_CD] = np.exp(lg[hd] * 128.0)
    j = idx % 32
    invf = 10000.0 ** (-(2.0 * (j % 16)) / 32.0)
    c[:, C_INVF] = invf
    c[:, C_SGN] = np.where(j < 16, -1.0, 1.0)
    c[:, C_ONES:C_ONES + 128] = 1.0
    return c.astype(np.float32)


def _win_perm():
    o_q, o_k, o_v, o_g, o_gu, o_gv, o_cb, o_cc, o_cx, o_code = 0, 128, 256, 512, 768, 1024, 1280, 1536, 1792, 2048
    r = np.arange(128)
    j = r % 32
    perm = (r // 32) * 32 + (j + 16) % 32
    cols = np.concatenate([
        o_q + r, o_q + perm, o_k + r, o_k + perm,
        o_g + np.arange(256), o_gu + np.arange(256),
        o_cb + np.arange(256), o_cc + np.arange(256), o_cx + np.arange(256),
        o_code + np.arange(384),
        o_v + np.arange(256), o_gv + np.arange(256)])
    assert cols.shape[0] == WIN_ALL
    return cols


class Prog:
    def __init__(self, nc, es):
        self.nc = nc
        self.es = es
        self.lw = {}
        self.rd = {}
        self.engs = {}
        for name, eng in (("pe", nc.tensor), ("act", nc.scalar), ("dve", nc.vector), ("pool", nc.gpsimd), ("sp", nc.sync)):
            sem = es.enter_context(nc.semaphore("sem_" + name))
            self.engs[name] = dict(eng=eng, sem=sem, cnt=0, waited={}, name=name)
        self.dsems = {}
        self.nwait = 0
        self.ninst = 0

    def dsem(self, name):
        if name not in self.dsems:
            self.dsems[name] = dict(sem=self.es.enter_context(self.nc.semaphore("d_" + name)), tot=0)
        return self.dsems[name]

    def _deps(self, en, reads, writes):
        e = self.engs[en]
        deps = []
        for k in reads:
            t = self.lw.get(k)
            if t is not None:
                deps.append(t)
        for k in writes:
            t = self.lw.get(k)
            if t is not None:
                deps.append(t)
            for t in self.rd.get(k, {}).values():
                deps.append(t)
        for (sem, val, owner) in deps:
            if en == "pe" and owner == "pe":
                continue
            sid = id(sem)
            if e["waited"].get(sid, 0) >= val:
                continue
            e["eng"].wait_ge(sem, val)
            e["waited"][sid] = val
            self.nwait += 1

    def _mark(self, tok, en, reads, writes):
        for k in reads:
            self.rd.setdefault(k, {})[en] = tok
        for k in writes:
            self.lw[k] = tok
            self.rd[k] = {}

    def op(self, en, fn, reads=(), writes=()):
        e = self.engs[en]
        self._deps(en, reads, writes)
        inst = fn(e["eng"])
        e["cnt"] += 1
        inst.then_inc(e["sem"], 1)
        self.ninst += 1
        self._mark((e["sem"], e["cnt"], en), en, reads, writes)

    def dma(self, q, semname, items, reads=(), writes=()):
        e = self.engs[q]
        ds = self.dsem(semname)
        self._deps(q, reads, writes)
        if ds["tot"] > 0 and e["waited"].get(id(ds["sem"]), 0) < ds["tot"]:
            e["eng"].wait_ge(ds["sem"], ds["tot"])
            e["waited"][id(ds["sem"])] = ds["tot"]
        for (o, i) in items:
            inst = e["eng"].dma_start(out=o, in_=i)
            ds["tot"] += 16
            inst.then_inc(ds["sem"], 16)
            self.ninst += 1
        self._mark((ds["sem"], ds["tot"], "dma_" + semname), "dma_" + semname + q, reads, writes)

    def barrier(self):
        toks = [(e["sem"], e["cnt"]) for e in self.engs.values() if e["cnt"] > 0]
        toks += [(d["sem"], d["tot"]) for d in self.dsems.values() if d["tot"] > 0]
        for e in self.engs.values():
            for (sem, val) in toks:
                if sem is e["sem"]:
                    continue
                sid = id(sem)
                if e["waited"].get(sid, 0) >= val:
                    continue
                e["eng"].wait_ge(sem, val)
                e["waited"][sid] = val

    def finish(self, q):
        e = self.engs[q]
        for d in self.dsems.values():
            if d["tot"] > 0:
                e["eng"].wait_ge(d["sem"], d["tot"])
        for o in self.engs.values():
            if o is not e and o["cnt"] > 0:
                e["eng"].wait_ge(o["sem"], o["cnt"])


def build(NB=4, NL=DEPTH, dbg=False):
    nc = bass.Bass("TRN2", target_bir_lowering=False)
    es = contextlib.ExitStack()
    P = Prog(nc, es)

    def din(name, shape, dt=F32):
        return nc.dram_tensor(name, list(shape), dt, kind="ExternalInput").ap()

    x_d = din("x", [NB, SEQ, D])
    cT_d = din("cT", [128, 8, NB])
    pos_d = din("pos", [NB, SEQ], I32)
    wada_d = din("w_ada", [DEPTH, D, 6 * D])
    win_d = din("w_in", [DEPTH, D, WIN_ALL])
    vecs_d = din("vecs", [NVEC, 128])
    const_d = din("consts", [128, NCONST])
    lng_d = din("gmlp_ln_g", [DEPTH, 256])
    lnb_d = din("gmlp_ln_b", [DEPTH, 256])
    wsp_d = din("w_spatial", [DEPTH, 4, 128, 128])
    bsp_d = din("b_spatial", [DEPTH, 4, 128])
    wbr_d = din("w_branch", [DEPTH, 768, D])
    wg_d = din("w_gate_up", [DEPTH, 384, D])
    wout_d = din("w_out", [DEPTH, D, D])
    wr_d = din("w_router", [DEPTH, D, E])
    br_d = din("b_router", [DEPTH, E])
    wgu_d = din("w_gu", [DEPTH, E, D, 2 * DFF])
    bgu_d = din("b_gu", [DEPTH, E, 2 * DFF])
    wd_d = din("w_down", [DEPTH, E, DFF, D])
    bd_d = din("b_down", [DEPTH, E, D])
    ind_d = din("ind", [1, 2 * DFF])
    out_d = nc.dram_tensor("out", [NB, SEQ, D], F32, kind="ExternalOutput").ap()

    def sb(name, shape, dt=F32):
        return es.enter_context(nc.sbuf_tensor(name, list(shape), dt))

    XT = sb("XT", [128, 8, UNIT])
    h2T = sb("h2T", [128, 8, UNIT], BF16)
    CT = sb("CT", [128, NCONST])
    identb = sb("identb", [128, 128], BF16)
    vecT = sb("vecT", [128, NVEC])
    adaT = sb("adaT", [128, NL, 48, NB])
    dsc = sb("dsc", [128, NL, NB, 4, 8])
    cosT = sb("cosT", [128, UNIT])
    sinT = sb("sinT", [128, UNIT])
    wgt = sb("wgt", [128, UNIT // 128, E])
    state = sb("state", [128, NL, 256])
    state_bf = sb("state_bf", [128, NL, 256], BF16)
    zhalo = sb("zhalo", [128, NL, 2, 2])
    lngb = sb("lngb", [128, 2, 256])
    WmT = sb("WmT", [128, 4, 128], BF16)
    bsp_row = sb("bsp_row", [1, 512], BF16)
    brt_row = sb("brt_row", [1, E], BF16)
    Wr = sb("Wr", [128, 8, E], BF16)
    bdn = sb("bdn", [E, D], BF16)
    ones_bf = sb("ones_bf", [2, 128], BF16)
    small = sb("small", [128, 64])
    Win = sb("Win", [128, 8, WIN_ALL], BF16)
    Wbr = sb("Wbr", [128, 6, D], BF16)
    Wg = sb("Wg", [128, 3, D], BF16)
    Wout = sb("Wout", [128, 8, D], BF16)
    ARENA = 46 * 1024 // 4
    arena = sb("arena", [128, ARENA])
    apos = [0]

    def carve(n_elems, dt=F32):
        nb = n_elems * (4 if dt in (F32, I32) else 2)
        nw = (nb + 3) // 4
        a = apos[0]
        apos[0] += nw
        assert apos[0] <= ARENA, ("arena overflow", apos[0], ARENA)
        v = arena[:, a:a + nw]
        return v if dt == F32 else v.bitcast(dt)

    apos[0] = 0
    hT = carve(8 * TM, BF16).rearrange("p (k t) -> p k t", k=8)
    rskT = carve(6 * TM, BF16).rearrange("p (k t) -> p k t", k=6)
    codeT = carve(3 * TM, BF16).rearrange("p (k t) -> p k t", k=3)
    qr = carve(TM, BF16)
    kr = carve(TM, BF16)
    qxi = carve(TM, BF16)
    kmask = carve(4 * TM, BF16).rearrange("p (k t) -> p k t", k=4)
    f_a = carve(2 * TM)
    f_b = carve(2 * TM)
    f_c = carve(2 * TM)
    f_d = carve(2 * TM)
    f_e = carve(2 * TM)
    gsil = carve(2 * TM).rearrange("p (k t) -> p k t", k=2)
    uT = carve(2 * TM).rearrange("p (k t) -> p k t", k=2)
    zbuf = carve(2 * (TM + 2)).rearrange("p (k t) -> p k t", k=2)
    vtok = carve(256, BF16)
    vln = carve(256, BF16)
    Sd = carve(512, BF16)
    kz = carve(128, BF16)
    t_a = carve(256)
    t_b = carve(256)
    t_c = carve(256)
    xstage = carve(1024)
    mix_end = apos[0]
    apos[0] = 0
    Wgu = [carve(8 * 512, BF16).rearrange("p (k f) -> p k f", k=8) for _ in range(2)]
    Wd = [carve(2 * D, BF16).rearrange("p (k f) -> p k f", k=2) for _ in range(2)]
    bgu = [carve(512, BF16) for _ in range(2)]
    NCH = 2
    cg = [carve(256) for _ in range(NCH)]
    csig = [carve(256) for _ in range(NCH)]
    cu = [carve(256) for _ in range(NCH)]
    cp_ = [carve(256) for _ in range(NCH)]
    cact = [carve(256, BF16) for _ in range(4)]
    actT = [carve(2 * TE, BF16).rearrange("p (k t) -> p k t", k=2) for _ in range(2)]
    wgtT = carve(TE)
    rt_a = carve(E)
    rt_b = carve(E)
    top8 = carve(8)
    moe_end = apos[0]

    banks = [es.enter_context(nc.psum_tensor("ps%d" % i, [128, 512], F32)) for i in range(8)]
    bk = [0]

    def psn():
        i = bk[0] % 8
        bk[0] += 1
        return banks[i], "ps%d" % i

    op, dma = P.op, P.dma
    ident = CT[:, C_ID:C_ID + 128]
    ones_f = CT[:, C_ONES:C_ONES + 128]

    dma("sp", "const", [(CT[:], const_d)], writes=["CT"])
    op("dve", lambda v: v.tensor_copy(out=identb[:], in_=ident), reads=["CT"], writes=["identb"])
    op("dve", lambda v: v.memset(ones_bf[:], 1.0), writes=["ones_bf"])
    for r in range(NVEC // 128):
        dma("sp", "setup", [(f_a[:, 0:128], vecs_d[r * 128:(r + 1) * 128, :])], writes=["f_a"])
        pb, pk = psn()
        op("pe", lambda t: t.transpose(out=pb[:, 0:128], in_=f_a[:, 0:128], identity=ident), reads=["f_a", "CT"], writes=[pk])
        op("dve", lambda v: v.tensor_copy(out=vecT[:, r * 128:(r + 1) * 128], in_=pb[:, 0:128]), reads=[pk], writes=["vecT"])
    cact_t = t_a[:, 0:8 * NB].rearrange("p (k b) -> p k b", k=8)
    csg = t_b[:, 0:8 * NB].rearrange("p (k b) -> p k b", k=8)
    cbf = vtok[:, 0:8 * NB].rearrange("p (k b) -> p k b", k=8)
    dma("sp", "setup", [(cact_t, cT_d)], writes=["t_a"])
    op("act", lambda a: a.activation(out=csg, in_=cact_t, func=AF.Sigmoid), reads=["t_a"], writes=["t_b"])
    op("dve", lambda v: v.tensor_tensor(out=cbf, in0=cact_t, in1=csg, op=ALU.mult), reads=["t_a", "t_b"], writes=["vtok"])
    WA = Win[:, :, 0:1536]
    for l in range(NL):
        for pc in range(4):
            dma("pool", "wada", [(WA, wada_d[l, :, pc * 1536:(pc + 1) * 1536].rearrange("(k p) n -> p k n", p=128))], writes=["Win"])
            for jj in range(12):
                j = pc * 12 + jj
                pb, pk = psn()
                for kc in range(8):
                    op("pe", lambda t: t.matmul(pb[:, 0:NB], lhsT=WA[:, kc, jj * 128:(jj + 1) * 128], rhs=cbf[:, kc, :],
                                                start=(kc == 0), stop=(kc == 7)), reads=["Win", "vtok"], writes=[pk])
                op("dve", lambda v: v.tensor_scalar(out=adaT[:, l, j, :], in0=pb[:, 0:NB], scalar1=vecT[:, V_BADA + l * 48 + j:V_BADA + l * 48 + j + 1],
                                                    scalar2=None, op0=ALU.add), reads=[pk, "vecT"], writes=["adaT"])
    for l in range(NL):
        for b in range(NB):
            op("dve", lambda v: v.tensor_scalar(out=dsc[:, l, b, 0, :], in0=adaT[:, l, 8:16, b], scalar1=1.0, scalar2=None, op0=ALU.add), reads=["adaT"], writes=["dsc"])
            op("dve", lambda v: v.tensor_scalar(out=dsc[:, l, b, 1, :], in0=adaT[:, l, 16:24, b], scalar1=1.0, scalar2=1.0 / ALPHA, op0=ALU.add, op1=ALU.mult), reads=["adaT"], writes=["dsc"])
            op("dve", lambda v: v.tensor_scalar(out=dsc[:, l, b, 2, :], in0=adaT[:, l, 32:40, b], scalar1=1.0, scalar2=None, op0=ALU.add), reads=["adaT"], writes=["dsc"])
            op("dve", lambda v: v.tensor_scalar(out=dsc[:, l, b, 3, :], in0=adaT[:, l, 40:48, b], scalar1=1.0, scalar2=1.0 / ALPHA, op0=ALU.add, op1=ALU.mult), reads=["adaT"], writes=["dsc"])

    def col(ap2d, j):
        return ap2d[:, j:j + 1]

    def gelu_tanh(src_ps, srck, dst, dstk, n, tmp1, tmp1k, tmp2, tmp2k):
        op("act", lambda a: a.activation(out=tmp1[:, 0:n], in_=src_ps, func=AF.Square), reads=[srck], writes=[tmp1k])
        op("dve", lambda v: v.tensor_scalar(out=tmp1[:, 0:n], in0=tmp1[:, 0:n], scalar1=0.044715, scalar2=1.0, op0=ALU.mult, op1=ALU.add), reads=[tmp1k], writes=[tmp1k])
        op("dve", lambda v: v.tensor_tensor(out=tmp1[:, 0:n], in0=src_ps, in1=tmp1[:, 0:n], op=ALU.mult), reads=[srck, tmp1k], writes=[tmp1k])
        op("act", lambda a: a.activation(out=tmp2[:, 0:n], in_=tmp1[:, 0:n], func=AF.Sigmoid, scale=1.5957691216057308), reads=[tmp1k], writes=[tmp2k])
        op("dve", lambda v: v.tensor_tensor(out=dst, in0=src_ps, in1=tmp2[:, 0:n], op=ALU.mult), reads=[srck, tmp2k], writes=[dstk])

    def rstd_from_var(var_ap, out_ap, eps, rk, wk):
        op("dve", lambda v: v.tensor_scalar(out=out_ap, in0=var_ap, scalar1=float(eps), scalar2=None, op0=ALU.add), reads=rk, writes=wk)
        op("act", lambda a: a.activation(out=out_ap, in_=out_ap, func=AF.Sqrt), reads=wk, writes=wk)
        op("dve", lambda v: v.reciprocal(out=out_ap, in_=out_ap), reads=wk, writes=wk)

    def load_mixer_weights(l):
        wl = lambda ap: ap.rearrange("(k p) n -> p k n", p=128)
        dma("pool", "wmix", [(Win[:, :, 0:1344], wl(win_d[l, :, 0:1344])), (Win[:, :, 1344:2688], wl(win_d[l, :, 1344:2688])), (Wbr[:], wl(wbr_d[l])), (Wg[:], wl(wg_d[l])), (Wout[:], wl(wout_d[l]))],
            writes=["Win", "Wbr", "Wg", "Wout"])
    first_mixer = [True]

    for b in range(NB):
        for hf in range(SEQ // UNIT):
            tok0 = hf * UNIT
            P.barrier()
            posi = xstage.bitcast(I32)
            dma("sp", "setup", [(posi, pos_d[b:b + 1, tok0:tok0 + UNIT].partition_broadcast(128))], writes=["xstage"])
            tmpI = hT.rearrange("p k t -> p (k t)").bitcast(I32)
            op("dve", lambda v: v.tensor_copy(out=sinT[:], in_=posi), reads=["xstage"], writes=["sinT"])
            op("dve", lambda v: v.tensor_scalar(out=sinT[:], in0=sinT[:], scalar1=CT[:, C_INVF:C_INVF + 1], scalar2=None, op0=ALU.mult), reads=["sinT", "CT"], writes=["sinT"])
            op("dve", lambda v: v.tensor_scalar(out=cosT[:], in0=sinT[:], scalar1=PI / 2, scalar2=None, op0=ALU.add), reads=["sinT"], writes=["cosT"])
            for (tab, tk, sgn) in ((sinT, "sinT", True), (cosT, "cosT", False)):
                ki = tmpI
                kf = xstage
                op("dve", lambda v: v.tensor_scalar(out=ki, in0=tab[:], scalar1=1.0 / (2 * PI), scalar2=None, op0=ALU.mult), reads=[tk], writes=["hT"])
                op("dve", lambda v: v.tensor_copy(out=kf, in_=ki), reads=["hT", "sinT"], writes=["xstage"])
                op("dve", lambda v: v.scalar_tensor_tensor(out=tab[:], in0=kf, scalar=-2 * PI, in1=tab[:], op0=ALU.mult, op1=ALU.add), reads=["xstage", tk], writes=[tk])
                op("dve", lambda v: v.tensor_scalar(out=kf, in0=tab[:], scalar1=PI, scalar2=2 * PI, op0=ALU.is_gt, op1=ALU.mult), reads=[tk], writes=["xstage"])
                op("dve", lambda v: v.tensor_tensor(out=tab[:], in0=tab[:], in1=kf, op=ALU.subtract), reads=[tk, "xstage"], writes=[tk])
                op("dve", lambda v: v.tensor_scalar(out=kf, in0=tab[:], scalar1=-PI, scalar2=2 * PI, op0=ALU.is_lt, op1=ALU.mult), reads=[tk], writes=["xstage"])
                op("dve", lambda v: v.tensor_tensor(out=tab[:], in0=tab[:], in1=kf, op=ALU.add), reads=[tk, "xstage"], writes=[tk])
                op("dve", lambda v: v.tensor_scalar(out=tab[:], in0=tab[:], scalar1=-PI, scalar2=PI, op0=ALU.max, op1=ALU.min), reads=[tk], writes=[tk])
                op("act", lambda a: a.activation(out=tab[:], in_=tab[:], func=AF.Sin), reads=[tk], writes=[tk])
                if sgn:
                    op("dve", lambda v: v.tensor_scalar(out=tab[:], in0=tab[:], scalar1=CT[:, C_SGN:C_SGN + 1], scalar2=None, op0=ALU.mult), reads=[tk, "CT"], writes=[tk])
            for c in range(UNIT // 128):
                dma("sp", "xin", [(xstage, x_d[b, tok0 + c * 128:tok0 + (c + 1) * 128, :])], writes=["xstage"])
                for g4 in range(2):
                    pb, pk = psn()
                    for q in range(4):
                        kc = g4 * 4 + q
                        op("pe", lambda t: t.transpose(out=pb[:, q * 128:(q + 1) * 128], in_=xstage[:, kc * 128:(kc + 1) * 128], identity=ident),
                           reads=["xstage", "CT"], writes=[pk])
                    op("act", lambda a: a.activation(out=XT[:, g4 * 4:(g4 + 1) * 4, c * 128:(c + 1) * 128], in_=pb[:].rearrange("p (q t) -> p q t", q=4), func=AF.Identity),
                       reads=[pk], writes=["XT"])
            if hf == 0:
                op("dve", lambda v: v.memset(state[:], 0.0), writes=["state"])
                op("dve", lambda v: v.memset(state_bf[:], 0.0), writes=["state_bf"])
                op("dve", lambda v: v.memset(zhalo[:], 0.0), writes=["zhalo"])

            for l in range(NL):
                P.barrier()
                if first_mixer[0]:
                    load_mixer_weights(l)
                    first_mixer[0] = False
                last_unit = (b == NB - 1 and hf == SEQ // UNIT - 1)
                next_mixer_weights = (l + 1) if l + 1 < NL else (None if last_unit else 0)
                if True:
                    dma("sp", "setup", [(lngb[:, 0, :], lng_d[l:l + 1, :].partition_broadcast(128)), (lngb[:, 1, :], lnb_d[l:l + 1, :].partition_broadcast(128))], writes=["lngb"])
                    dma("pool", "wsm", [(bsp_row[:], bsp_d[l:l + 1].rearrange("o g c -> o (g c)")), (brt_row[:], br_d[l:l + 1, :]),
                                        (Wr[:], wr_d[l].rearrange("(k p) n -> p k n", p=128)), (bdn[:], bd_d[l])],
                        writes=["bsp_row", "brt_row", "Wr", "bdn"])
                    for g in range(4):
                        dma("sp", "setup", [(t_a[:, 0:128], wsp_d[l, g])], writes=["t_a"])
                        pb, pk = psn()
                        op("pe", lambda t: t.transpose(out=pb[:, 0:128], in_=t_a[:, 0:128], identity=ident), reads=["t_a", "CT"], writes=[pk])
                        op("dve", lambda v: v.tensor_tensor(out=WmT[:, g, :], in0=pb[:, 0:128], in1=CT[:, C_CAUS:C_CAUS + 128], op=ALU.mult), reads=[pk, "CT"], writes=["WmT"])
                A1 = dsc[:, l, b, 0, :]
                G1 = dsc[:, l, b, 1, :]
                A2 = dsc[:, l, b, 2, :]
                G2 = dsc[:, l, b, 3, :]
                for tj in range(UNIT // TM):
                    ts = slice(tj * TM, (tj + 1) * TM)
                    for kc in range(8):
                        op("act", lambda a: a.activation(out=hT[:, kc, :], in_=XT[:, kc, ts], func=AF.Identity, scale=col(A1, kc), bias=adaT[:, l, kc, b:b + 1]),
                           reads=["XT", "dsc", "adaT"], writes=["hT"])

                    def proj(c0, nch):
                        pb, pk = psn()
                        for q in range(nch):
                            for kc in range(8):
                                op("pe", lambda t: t.matmul(pb[:, q * TM:(q + 1) * TM], lhsT=Win[:, kc, (c0 + q) * 128:(c0 + q + 1) * 128], rhs=hT[:, kc, :],
                                                            start=(kc == 0), stop=(kc == 7)), reads=["Win", "hT"], writes=[pk])
                        return pb, pk
                    for (c0, dst, dk) in ((0, qr, "qr"), (2, kr, "kr")):
                        pb, pk = proj(c0, 2)
                        op("dve", lambda v: v.tensor_tensor(out=f_a[:, 0:TM], in0=pb[:, 0:TM], in1=cosT[:, ts], op=ALU.mult), reads=[pk, "cosT"], writes=["f_a"])
                        op("dve", lambda v: v.tensor_tensor(out=f_b[:, 0:TM], in0=pb[:, TM:2 * TM], in1=sinT[:, ts], op=ALU.mult), reads=[pk, "sinT"], writes=["f_b"])
                        op("dve", lambda v: v.tensor_tensor(out=dst, in0=f_a[:, 0:TM], in1=f_b[:, 0:TM], op=ALU.add), reads=["f_a", "f_b"], writes=[dk])
                    op("dve", lambda v: v.tensor_tensor(out=qxi, in0=qr, in1=CT[:, C_XI:C_XI + TM], op=ALU.mult), reads=["qr", "CT"], writes=["qxi"])
                    for h in range(4):
                        op("act", lambda a: a.activation(out=kmask[:, h, :], in_=kr, func=AF.Identity, scale=CT[:, C_HM + h:C_HM + h + 1]), reads=["kr", "CT"], writes=["kmask"])
                    pb, pk = proj(4, 2)
                    op("act", lambda a: a.activation(out=f_a, in_=pb[:], func=AF.Sigmoid), reads=[pk], writes=["f_a"])
                    op("dve", lambda v: v.tensor_tensor(out=gsil.rearrange("p k t -> p (k t)"), in0=pb[:], in1=f_a, op=ALU.mult), reads=[pk, "f_a"], writes=["gsil"])
                    pb, pk = proj(6, 2)
                    gelu_tanh(pb[:], pk, uT.rearrange("p k t -> p (k t)"), "uT", 2 * TM, f_b, "f_b", f_c, "f_c")
                    pcb, kcb = proj(8, 2)
                    pcc, kcc = proj(10, 2)
                    pcx, kcx = proj(12, 2)
                    op("act", lambda a: a.activation(out=f_a, in_=pcc[:], func=AF.Identity), reads=[kcc], writes=["f_a"])
                    for ch in range(2):
                        op("dve", lambda v: v.tensor_copy(out=zbuf[:, ch, 0:2], in_=zhalo[:, l, ch, :]), reads=["zhalo"], writes=["zbuf"])
                        op("dve", lambda v: v.tensor_tensor(out=zbuf[:, ch, 2:TM + 2], in0=pcx[:, ch * TM:(ch + 1) * TM], in1=f_a[:, ch * TM:(ch + 1) * TM], op=ALU.mult),
                           reads=[kcx, "f_a"], writes=["zbuf"])
                        op("dve", lambda v: v.tensor_copy(out=zhalo[:, l, ch, :], in_=zbuf[:, ch, TM:TM + 2]), reads=["zbuf"], writes=["zhalo"])
                        w0 = vecT[:, V_CONVW + (l * 3 + 0) * 2 + ch:V_CONVW + (l * 3 + 0) * 2 + ch + 1]
                        w1 = vecT[:, V_CONVW + (l * 3 + 1) * 2 + ch:V_CONVW + (l * 3 + 1) * 2 + ch + 1]
                        w2 = vecT[:, V_CONVW + (l * 3 + 2) * 2 + ch:V_CONVW + (l * 3 + 2) * 2 + ch + 1]
                        acc = f_d[:, ch * TM:(ch + 1) * TM]
                        op("dve", lambda v: v.tensor_scalar(out=acc, in0=zbuf[:, ch, 2:TM + 2], scalar1=w2, scalar2=None, op0=ALU.mult), reads=["zbuf", "vecT"], writes=["f_d"])
                        op("dve", lambda v: v.scalar_tensor_tensor(out=acc, in0=zbuf[:, ch, 1:TM + 1], scalar=w1, in1=acc, op0=ALU.mult, op1=ALU.add), reads=["zbuf", "vecT", "f_d"], writes=["f_d"])
                        op("dve", lambda v: v.scalar_tensor_tensor(out=acc, in0=zbuf[:, ch, 0:TM], scalar=w0, in1=acc, op0=ALU.mult, op1=ALU.add), reads=["zbuf", "vecT", "f_d"], writes=["f_d"])
                    op("dve", lambda v: v.tensor_tensor(out=rskT[:, 4:6, :].rearrange("p k t -> p (k t)"), in0=pcb[:], in1=f_d, op=ALU.mult), reads=[kcb, "f_d"], writes=["rskT"])
                    pb, pk = proj(14, 2)
                    op("act", lambda a: a.activation(out=codeT[:, 0:2, :].rearrange("p k t -> p (k t)"), in_=pb[:], func=AF.Identity), reads=[pk], writes=["codeT"])
                    pb, pk = proj(16, 1)
                    op("act", lambda a: a.activation(out=codeT[:, 2, :], in_=pb[:, 0:TM], func=AF.Identity), reads=[pk], writes=["codeT"])
                    for cc_ in range(TM // 128):
                        cs = slice(cc_ * 128, (cc_ + 1) * 128)
                        ptm, ktm = psn()
                        for kc in range(8):
                            op("pe", lambda t: t.matmul(ptm[:], lhsT=hT[:, kc, cs], rhs=Win[:, kc, WIN_FM:WIN_ALL], start=(kc == 0), stop=(kc == 7)), reads=["hT", "Win"], writes=[ktm])
                        op("act", lambda a: a.activation(out=vtok, in_=ptm[:, 0:256], func=AF.Identity), reads=[ktm], writes=["vtok"])
                        gelu_tanh(ptm[:, 256:512], ktm, t_a, "t_a", 256, t_b, "t_b", t_c, "t_c")
                        op("dve", lambda v: v.bn_stats(out=small[:, 0:6], in_=t_a), reads=["t_a"], writes=["small"])
                        op("dve", lambda v: v.bn_aggr(out=small[:, 8:10], in_=small[:, 0:6]), reads=["small"], writes=["small"])
                        rstd_from_var(small[:, 9:10], small[:, 10:11], LN_EPS, ["small"], ["small"])
                        op("dve", lambda v: v.tensor_scalar(out=t_a, in0=t_a, scalar1=small[:, 8:9], scalar2=small[:, 10:11], op0=ALU.subtract, op1=ALU.mult), reads=["t_a", "small"], writes=["t_a"])
                        op("dve", lambda v: v.tensor_tensor(out=t_a, in0=t_a, in1=lngb[:, 0, :], op=ALU.mult), reads=["t_a", "lngb"], writes=["t_a"])
                        op("dve", lambda v: v.tensor_tensor(out=vln, in0=t_a, in1=lngb[:, 1, :], op=ALU.add), reads=["t_a", "lngb"], writes=["vln"])
                        pz, kzk = psn()
                        for g in range(4):
                            op("pe", lambda t: t.matmul(pz[:, g * 64:(g + 1) * 64], lhsT=WmT[:, g, :], rhs=vln[:, g * 64:(g + 1) * 64], start=True, stop=False), reads=["WmT", "vln"], writes=[kzk])
                            op("pe", lambda t: t.matmul(pz[:, g * 64:(g + 1) * 64], lhsT=bsp_row[0:1, g * 128:(g + 1) * 128], rhs=ones_bf[0:1, 0:64], start=False, stop=True),
                               reads=["bsp_row", "ones_bf"], writes=[kzk])
                        op("act", lambda a: a.activation(out=t_b, in_=pz[:, 0:256], func=AF.Identity), reads=[kzk], writes=["t_b"])
                        ptr, ktr = psn()
                        for q in range(2):
                            op("pe", lambda t: t.transpose(out=ptr[:, q * 128:(q + 1) * 128], in_=t_b[:, q * 128:(q + 1) * 128], identity=ident), reads=["t_b", "CT"], writes=[ktr])
                        op("dve", lambda v: v.tensor_tensor(out=rskT[:, 2:4, cs], in0=ptr[:, 0:256].rearrange("p (q t) -> p q t", q=2), in1=uT[:, :, cs], op=ALU.mult), reads=[ktr, "uT"], writes=["rskT"])
                        psc, ksc = psn()
                        for h in range(4):
                            op("pe", lambda t: t.matmul(psc[:, h * 128:(h + 1) * 128], lhsT=kmask[:, h, cs], rhs=qr[:, cs], start=True, stop=True), reads=["kmask", "qr"], writes=[ksc])
                        op("dve", lambda v: v.tensor_tensor(out=Sd, in0=psc[:], in1=CT[:, C_DECAY:C_DECAY + 512], op=ALU.mult), reads=[ksc, "CT"], writes=["Sd"])
                        po, ko = psn()
                        op("pe", lambda t: t.matmul(po[:, 0:256], lhsT=qxi[:, cs], rhs=state_bf[:, l, :], start=True, stop=False), reads=["qxi", "state_bf"], writes=[ko])
                        for h in range(4):
                            op("pe", lambda t: t.matmul(po[:, h * 64:(h + 1) * 64], lhsT=Sd[:, h * 128:(h + 1) * 128], rhs=vtok[:, h * 64:(h + 1) * 64], start=False, stop=(h == 3)),
                               reads=["Sd", "vtok"], writes=[ko])
                        for h in range(4):
                            op("dve", lambda v: v.bn_stats(out=small[:, 16 + h * 6:22 + h * 6], in_=po[:, h * 64:(h + 1) * 64]), reads=[ko], writes=["small"])
                        for h in range(4):
                            op("dve", lambda v: v.bn_aggr(out=small[:, 40 + 2 * h:42 + 2 * h], in_=small[:, 16 + h * 6:22 + h * 6]), reads=["small"], writes=["small"])
                        sm3 = small[:, 40:48].rearrange("p (h t) -> p h t", t=2)
                        rstd_from_var(sm3[:, :, 1], small[:, 48:52], LN_EPS, ["small"], ["small"])
                        for h in range(4):
                            op("dve", lambda v: v.tensor_scalar(out=t_c[:, h * 64:(h + 1) * 64], in0=po[:, h * 64:(h + 1) * 64], scalar1=small[:, 40 + 2 * h:41 + 2 * h],
                                                                scalar2=small[:, 48 + h:49 + h], op0=ALU.subtract, op1=ALU.mult), reads=[ko, "small"], writes=["t_c"])
                        ptr, ktr = psn()
                        for q in range(2):
                            op("pe", lambda t: t.transpose(out=ptr[:, q * 128:(q + 1) * 128], in_=t_c[:, q * 128:(q + 1) * 128], identity=ident), reads=["t_c", "CT"], writes=[ktr])
                        op("dve", lambda v: v.tensor_tensor(out=rskT[:, 0:2, cs], in0=ptr[:, 0:256].rearrange("p (q t) -> p q t", q=2), in1=gsil[:, :, cs], op=ALU.mult), reads=[ktr, "gsil"], writes=["rskT"])
                        pkt, kkt = psn()
                        pkt_b = pkt[:].bitcast(BF16)
                        op("pe", lambda t: t.transpose(out=pkt_b[:, 0:128], in_=kr[:, cs], identity=identb[:]), reads=["kr", "identb"], writes=[kkt])
                        op("dve", lambda v: v.tensor_tensor(out=kz, in0=pkt_b[:, 0:128], in1=CT[:, C_ZETA:C_ZETA + 128], op=ALU.mult), reads=[kkt, "CT"], writes=["kz"])
                        pkv, kkv = psn()
                        op("pe", lambda t: t.matmul(pkv[:, 0:256], lhsT=kz, rhs=vtok, start=True, stop=True), reads=["kz", "vtok"], writes=[kkv])
                        op("dve", lambda v: v.tensor_tensor(out=t_b, in0=pkv[:, 0:256], in1=CT[:, C_BD:C_BD + 256], op=ALU.mult), reads=[kkv, "CT"], writes=["t_b"])
                        op("dve", lambda v: v.scalar_tensor_tensor(out=state[:, l, :], in0=state[:, l, :], scalar=CT[:, C_CD:C_CD + 1], in1=t_b, op0=ALU.mult, op1=ALU.add),
                           reads=["state", "CT", "t_b"], writes=["state"])
                        op("act", lambda a: a.activation(out=state_bf[:, l, :], in_=state[:, l, :], func=AF.Identity), reads=["state"], writes=["state_bf"])
                    mergedT = hT
                    for dc in range(8):
                        dsl = slice(dc * 128, (dc + 1) * 128)
                        for i in range(3):
                            py, ky = psn()
                            for kk in range(2):
                                op("pe", lambda t: t.matmul(py[:, 0:TM], lhsT=Wbr[:, i * 2 + kk, dsl], rhs=rskT[:, i * 2 + kk, :], start=(kk == 0), stop=(kk == 1)), reads=["Wbr", "rskT"], writes=[ky])
                            op("pe", lambda t: t.matmul(py[:, TM:2 * TM], lhsT=Wg[:, i, dsl], rhs=codeT[:, i, :], start=True, stop=True), reads=["Wg", "codeT"], writes=[ky])
                            bg = vecT[:, V_BGATE + (l * 3 + i) * 8 + dc:V_BGATE + (l * 3 + i) * 8 + dc + 1]
                            op("act", lambda a: a.activation(out=f_a[:, 0:TM], in_=py[:, TM:2 * TM], func=AF.Sigmoid, bias=bg), reads=[ky, "vecT"], writes=["f_a"])
                            if i == 0:
                                op("dve", lambda v: v.tensor_tensor(out=f_b[:, 0:TM], in0=py[:, 0:TM], in1=f_a[:, 0:TM], op=ALU.mult), reads=[ky, "f_a"], writes=["f_b"])
                            else:
                                op("dve", lambda v: v.tensor_tensor(out=f_c[:, 0:TM], in0=py[:, 0:TM], in1=f_a[:, 0:TM], op=ALU.mult), reads=[ky, "f_a"], writes=["f_c"])
                                if i == 1:
                                    op("dve", lambda v: v.tensor_tensor(out=f_b[:, 0:TM], in0=f_b[:, 0:TM], in1=f_c[:, 0:TM], op=ALU.add), reads=["f_b", "f_c"], writes=["f_b"])
                                else:
                                    op("dve", lambda v: v.tensor_tensor(out=mergedT[:, dc, :], in0=f_b[:, 0:TM], in1=f_c[:, 0:TM], op=ALU.add), reads=["f_b", "f_c"], writes=["hT"])
                    for dc in range(8):
                        pm, km = psn()
                        for kc in range(8):
                            op("pe", lambda t: t.matmul(pm[:, 0:TM], lhsT=Wout[:, kc, dc * 128:(dc + 1) * 128], rhs=mergedT[:, kc, :], start=(kc == 0), stop=(kc == 7)), reads=["Wout", "hT"], writes=[km])
                        op("dve", lambda v: v.scalar_tensor_tensor(out=XT[:, dc, ts], in0=pm[:, 0:TM], scalar=col(G1, dc), in1=XT[:, dc, ts], op0=ALU.mult, op1=ALU.add),
                           reads=[km, "dsc", "XT"], writes=["XT"])
                    layer_norm_tile = None

                    def ln_tile(tsl, n, gvrow, bvrow, post):
                        ps1, k1 = psn()
                        for kc in range(8):
                            op("pe", lambda t: t.matmul(ps1[:, 0:n], lhsT=ones_f, rhs=XT[:, kc, tsl], start=(kc == 0), stop=(kc == 7)), reads=["CT", "XT"], writes=[k1])
                        ps2, k2 = psn()
                        for kc in range(8):
                            sq = (f_a, f_b)[kc % 2]
                            sqk = ("f_a", "f_b")[kc % 2]
                            op("act", lambda a: a.activation(out=sq[:, 0:n], in_=XT[:, kc, tsl], func=AF.Square), reads=["XT"], writes=[sqk])
                            op("pe", lambda t: t.matmul(ps2[:, 0:n], lhsT=ones_f, rhs=sq[:, 0:n], start=(kc == 0), stop=(kc == 7)), reads=["CT", sqk], writes=[k2])
                        mean = f_c[:, 0:n]
                        rstd = f_d[:, 0:n]
                        nmr = f_e[:, 0:n]
                        op("act", lambda a: a.activation(out=mean, in_=ps1[:, 0:n], func=AF.Identity, scale=1.0 / D), reads=[k1], writes=["f_c"])
                        op("dve", lambda v: v.tensor_tensor(out=nmr, in0=mean, in1=mean, op=ALU.mult), reads=["f_c"], writes=["f_e"])
                        op("dve", lambda v: v.scalar_tensor_tensor(out=rstd, in0=ps2[:, 0:n], scalar=1.0 / D, in1=nmr, op0=ALU.mult, op1=ALU.subtract), reads=[k2, "f_e"], writes=["f_d"])
                        rstd_from_var(rstd, rstd, LN_EPS / (ALPHA * ALPHA), ["f_d"], ["f_d"])
                        op("dve", lambda v: v.scalar_tensor_tensor(out=nmr, in0=mean, scalar=-1.0, in1=rstd, op0=ALU.mult, op1=ALU.mult), reads=["f_c", "f_d"], writes=["f_e"])
                        for kc in range(8):
                            xn = (f_a, f_b)[kc % 2][:, 0:n]
                            xk = ("f_a", "f_b")[kc % 2]
                            op("dve", lambda v: v.tensor_tensor(out=xn, in0=XT[:, kc, tsl], in1=rstd, op=ALU.mult), reads=["XT", "f_d"], writes=[xk])
                            op("dve", lambda v: v.tensor_tensor(out=xn, in0=xn, in1=nmr, op=ALU.add), reads=[xk, "f_e"], writes=[xk])
                            op("act", lambda a: a.activation(out=XT[:, kc, tsl], in_=xn, func=AF.Identity, scale=vecT[:, gvrow + kc:gvrow + kc + 1], bias=vecT[:, bvrow + kc:bvrow + kc + 1]),
                               reads=[xk, "vecT"], writes=["XT"])
                            if post is not None:
                                post(kc)
                    ln_tile(ts, TM, V_LN1G + l * 8, V_LN1B + l * 8,
                            lambda kc: op("act", lambda a: a.activation(out=h2T[:, kc, ts], in_=XT[:, kc, ts], func=AF.Identity, scale=col(A2, kc), bias=adaT[:, l, 24 + kc, b:b + 1]),
                                          reads=["XT", "dsc", "adaT"], writes=["h2T"]))

                P.barrier()
                NS = UNIT // 128
                for s in range(NS):
                    ss = slice(s * 128, (s + 1) * 128)
                    pr, kr_ = psn()
                    for kc in range(8):
                        op("pe", lambda t: t.matmul(pr[:, 0:E], lhsT=h2T[:, kc, ss], rhs=Wr[:, kc, :], start=(kc == 0), stop=False), reads=["h2T", "Wr"], writes=[kr_])
                    op("pe", lambda t: t.matmul(pr[:, 0:E], lhsT=ones_bf[0:1, :], rhs=brt_row[0:1, :], start=False, stop=True), reads=["ones_bf", "brt_row"], writes=[kr_])
                    op("dve", lambda v: v.tensor_copy(out=rt_a, in_=pr[:, 0:E]), reads=[kr_], writes=["rt_a"])
                    op("dve", lambda v: v.max(out=top8, in_=rt_a), reads=["rt_a"], writes=["top8"])
                    op("dve", lambda v: v.tensor_scalar(out=small[:, 56:57], in0=top8[:, 0:1], scalar1=-1.0, scalar2=None, op0=ALU.mult), reads=["top8"], writes=["small"])
                    op("act", lambda a: a.activation(out=rt_b, in_=rt_a, func=AF.Exp, bias=small[:, 56:57]), reads=["rt_a", "small"], writes=["rt_b"])
                    op("dve", lambda v: v.scalar_tensor_tensor(out=rt_b, in0=rt_a, scalar=top8[:, 3:4], in1=rt_b, op0=ALU.is_ge, op1=ALU.mult), reads=["rt_a", "top8", "rt_b"], writes=["rt_b"])
                    op("dve", lambda v: v.reduce_sum(out=small[:, 57:58], in_=rt_b, axis=mybir.AxisListType.X), reads=["rt_b"], writes=["small"])
                    op("dve", lambda v: v.reciprocal(out=small[:, 58:59], in_=small[:, 57:58]), reads=["small"], writes=["small"])
                    op("dve", lambda v: v.tensor_scalar(out=wgt[:, s, :], in0=rt_b, scalar1=small[:, 58:59], scalar2=None, op0=ALU.mult), reads=["rt_b", "small"], writes=["wgt"])
                for tt in range(UNIT // TE):
                    for s4 in range(4):
                        s = tt * 4 + s4
                        pw, kw = psn()
                        op("pe", lambda t: t.transpose(out=pw[0:E, 0:128], in_=wgt[:, s, :], identity=ident), reads=["wgt", "CT"], writes=[kw])
                        op("act", lambda a: a.activation(out=wgtT.bitcast(BF16)[0:E, s4 * 128:(s4 + 1) * 128], in_=pw[0:E, 0:128], func=AF.Identity), reads=[kw], writes=["wgtT"])
                    for dc in range(8):
                        pbd, kbd = psn()
                        op("pe", lambda t: t.matmul(pbd[:], lhsT=bdn[:, dc * 128:(dc + 1) * 128], rhs=wgtT.bitcast(BF16)[0:E, 0:TE], start=True, stop=True), reads=["bdn", "wgtT"], writes=[kbd])
                        xk = "XT%d_%d" % (tt, dc)
                        op("dve", lambda v: v.scalar_tensor_tensor(out=XT[:, dc, tt * TE:(tt + 1) * TE], in0=pbd[:], scalar=col(G2, dc), in1=XT[:, dc, tt * TE:(tt + 1) * TE], op0=ALU.mult, op1=ALU.add),
                           reads=[kbd, "dsc", "XT"], writes=["XT", xk])

                def load_gu(e):
                    sl = e % 2
                    dma("pool", "wexp%d" % sl,
                        [(Wgu[sl], wgu_d[l, e].rearrange("(k p) n -> p k n", p=128)),
                         (bgu[sl][0:1, :], bgu_d[l, e:e + 1, :])],
                        writes=["Wgu%d" % sl, "bgu%d" % sl])

                def load_d(e):
                    sl = e % 2
                    dma("pool", "wexd%d" % sl,
                        [(Wd[sl], wd_d[l, e].rearrange("(k p) n -> p k n", p=128))],
                        writes=["Wd%d" % sl])

                dma("pool", "wsm", [(bgu[0][1:2, :], ind_d), (bgu[1][1:2, :], ind_d)], writes=["bgu0", "bgu1"])
                NT = UNIT // TE
                units = [(e, tt) for e in range(E) for tt in range(NT)]
                nH = len(units) * 2
                pgc = [0]
                cic = [0]
                cac = [0]
                half_a = {}

                def stepA(H):
                    e, tt = units[H // 2]
                    sl = e % 2
                    if tt == 0 and H % 2 == 0 and e == 2:
                        nxt = next_mixer_weights
                        if nxt is not None:
                            load_mixer_weights(nxt)
                    subs = []
                    for j in range(2):
                        s = tt * 4 + (H % 2) * 2 + j
                        ss = slice(s * 128, (s + 1) * 128)
                        bi = pgc[0] % 4
                        pgc[0] += 1
                        pg, kg = banks[bi], "ps%d" % bi
                        for kc in range(8):
                            op("pe", lambda t: t.matmul(pg[:], lhsT=h2T[:, kc, ss], rhs=Wgu[sl][:, kc, :], start=(kc == 0), stop=False), reads=["h2T", "Wgu%d" % sl], writes=[kg])
                        op("pe", lambda t: t.matmul(pg[:], lhsT=ones_bf[0:2, :], rhs=bgu[sl][0:2, :], start=False, stop=True), reads=["ones_bf", "bgu%d" % sl], writes=[kg])
                        c3 = cic[0] % NCH
                        cic[0] += 1
                        c4 = cac[0] % 4
                        cac[0] += 1
                        subs.append((s, pg, kg, c3, c4))
                    for (s, pg, kg, c3, c4) in subs:
                        op("dve", lambda v: v.tensor_scalar(out=cg[c3], in0=pg[:, 0:256], scalar1=7.0, scalar2=None, op0=ALU.min), reads=[kg], writes=["cg%d" % c3])
                    for (s, pg, kg, c3, c4) in subs:
                        op("act", lambda a: a.activation(out=csig[c3], in_=cg[c3], func=AF.Sigmoid, scale=1.702), reads=["cg%d" % c3], writes=["cs%d" % c3])
                    for (s, pg, kg, c3, c4) in subs:
                        op("dve", lambda v: v.tensor_scalar(out=cu[c3], in0=pg[:, 256:512], scalar1=8.0, scalar2=-6.0, op0=ALU.min, op1=ALU.max), reads=[kg], writes=["cu%d" % c3])
                    for (s, pg, kg, c3, c4) in subs:
                        op("dve", lambda v: v.scalar_tensor_tensor(out=cp_[c3], in0=cg[c3], scalar=wgt[:, s, e:e + 1], in1=cu[c3], op0=ALU.mult, op1=ALU.mult),
                           reads=["cg%d" % c3, "cu%d" % c3, "wgt"], writes=["cp%d" % c3])
                    for (s, pg, kg, c3, c4) in subs:
                        op("pool", lambda g: g.tensor_tensor(out=cact[c4], in0=cp_[c3], in1=csig[c3], op=ALU.mult), reads=["cp%d" % c3, "cs%d" % c3], writes=["ca%d" % c4])
                    half_a[H] = [(s, c4) for (s, pg, kg, c3, c4) in subs]
                    if tt == NT - 1 and H % 2 == 1 and e + 2 < E:
                        load_gu(e + 2)

                def stepC(H):
                    e, tt = units[H // 2]
                    t_idx = H // 2
                    bi = 4 + t_idx % 2
                    pT_b = banks[bi][:].bitcast(BF16)
                    kT = "ps%d" % bi
                    for (s, c4) in half_a.pop(H):
                        s4 = s % 4
                        for fc in range(2):
                            op("pe", lambda t: t.transpose(out=pT_b[:, fc * TE + s4 * 128:fc * TE + (s4 + 1) * 128], in_=cact[c4][:, fc * 128:(fc + 1) * 128], identity=identb[:]),
                               reads=["ca%d" % c4, "identb"], writes=[kT])
                    if H % 2 == 1:
                        ai = t_idx % 2
                        op("act", lambda a: a.activation(out=actT[ai].rearrange("p k t -> p (k t)"), in_=pT_b[:, 0:2 * TE], func=AF.Identity), reads=[kT], writes=["actT%d" % ai])

                ydc = [0]

                def stepD(t_idx):
                    e, tt = units[t_idx]
                    sl = e % 2
                    ai = t_idx % 2
                    for dc in range(8):
                        bi = 6 + ydc[0] % 2
                        ydc[0] += 1
                        pyd, kyd = banks[bi], "ps%d" % bi
                        for fc in range(2):
                            op("pe", lambda t: t.matmul(pyd[:], lhsT=Wd[sl][:, fc, dc * 128:(dc + 1) * 128], rhs=actT[ai][:, fc, :], start=(fc == 0), stop=(fc == 1)),
                               reads=["Wd%d" % sl, "actT%d" % ai], writes=[kyd])
                        xk = "XT%d_%d" % (tt, dc)
                        op("dve", lambda v: v.scalar_tensor_tensor(out=XT[:, dc, tt * TE:(tt + 1) * TE], in0=pyd[:], scalar=col(G2, dc), in1=XT[:, dc, tt * TE:(tt + 1) * TE], op0=ALU.mult, op1=ALU.add),
                           reads=[kyd, "dsc", xk], writes=[xk])
                    if tt == NT - 1 and e + 2 < E:
                        load_d(e + 2)

                load_gu(0)
                load_d(0)
                load_gu(1)
                load_d(1)
                for H in range(nH + 3):
                    if H < nH:
                        stepA(H)
                    if 1 <= H <= nH:
                        stepC(H - 1)
                    if H >= 3 and H % 2 == 1 and (H - 3) // 2 < len(units):
                        stepD((H - 3) // 2)
                for tt in range(UNIT // TE):
                    for dc in range(8):
                        pass
                P.barrier()
                for tq in range(UNIT // TM):
                    ln_tile(slice(tq * TM, (tq + 1) * TM), TM, V_LN2G + l * 8, V_LN2B + l * 8, None)

            P.barrier()
            for c in range(UNIT // 128):
                for g4 in range(2):
                    pb, pk = psn()
                    for q in range(4):
                        kc = g4 * 4 + q
                        op("pe", lambda t: t.transpose(out=pb[:, q * 128:(q + 1) * 128], in_=XT[:, kc, c * 128:(c + 1) * 128], identity=ident), reads=["XT", "CT"], writes=[pk])
                    op("act", lambda a: a.activation(out=xstage[:, g4 * 512:(g4 + 1) * 512], in_=pb[:], func=AF.Identity), reads=[pk], writes=["xstage"])
                dma("sp", "xout", [(out_d[b, tok0 + c * 128:tok0 + (c + 1) * 128, :], xstage)], reads=["xstage"])
    P.finish("sp")
    print("instructions", P.ninst, "waits", P.nwait, "arena mix/moe", mix_end * 4, moe_end * 4)
    es.close()
    return nc


_CONSTS = None


def _prep(inputs, NB, cores):
    global _CONSTS
    if _CONSTS is None:
        _CONSTS = _consts()
    g = lambda k: np.asarray(inputs[k])
    L = DEPTH
    perm = _win_perm()
    w_in = np.ascontiguousarray(g("w_in")[:, :, perm])
    vecs = np.zeros((NVEC, 128), np.float32)
    vecs[V_BADA:V_BADA + 192] = g("b_ada").reshape(L * 48, 128)
    vecs[V_LN1G:V_LN1G + 32] = g("ln1_g").reshape(L * 8, 128)
    vecs[V_LN1B:V_LN1B + 32] = g("ln1_b").reshape(L * 8, 128)
    vecs[V_LN2G:V_LN2G + 32] = g("ln2_g").reshape(L * 8, 128)
    vecs[V_LN2B:V_LN2B + 32] = g("ln2_b").reshape(L * 8, 128)
    vecs[V_BGATE:V_BGATE + 96] = g("b_gate").reshape(L * 3 * 8, 128)
    vecs[V_CONVW:V_CONVW + 24] = g("conv_w").reshape(L * 3 * 2, 128)
    shared = {
        "w_ada": g("w_ada"), "w_in": w_in, "vecs": vecs, "consts": _CONSTS,
        "gmlp_ln_g": g("gmlp_ln_g"), "gmlp_ln_b": g("gmlp_ln_b"),
        "w_spatial": g("w_spatial"), "b_spatial": g("b_spatial"),
        "w_branch": np.ascontiguousarray(g("w_branch").reshape(L, 768, D)),
        "w_gate_up": np.ascontiguousarray(g("w_gate_up").reshape(L, 384, D)),
        "w_out": g("w_out"), "w_router": g("w_router"), "b_router": g("b_router"),
        "w_gu": g("w_gu"), "b_gu": g("b_gu"), "w_down": g("w_down"), "b_down": g("b_down"),
        "ind": np.concatenate([np.zeros((1, DFF), np.float32), np.ones((1, DFF), np.float32)], axis=1),
    }
    x = g("x")
    c = g("c")
    pos = g("positions").astype(np.int32)
    maps = []
    for i in range(cores):
        bs = slice(i * NB, (i + 1) * NB)
        cT = np.ascontiguousarray(c[bs].reshape(NB, 8, 128).transpose(2, 1, 0))
        m = dict(shared)
        m.update({"x": np.ascontiguousarray(x[bs]), "cT": cT, "pos": np.ascontiguousarray(pos[bs])})
        maps.append(m)
    return maps


def kernel(**inputs):
    NB = 32 // NCORES
    nc = build(NB=NB, NL=DEPTH)
    maps = _prep(inputs, NB, NCORES)
    res = run_bass_kernel_spmd(nc, maps, core_ids=list(range(NCORES)))
    out = np.concatenate([np.asarray(r["out"]) for r in res.results], axis=0)
    return out.astype(np.float32)
```

```python
import contextlib
import math
import numpy as np
import concourse.bass as bass
import concourse.mybir as mybir
from concourse.bass_utils import run_bass_kernel_spmd

F32 = mybir.dt.float32
BF16 = mybir.dt.bfloat16
I32 = mybir.dt.int32
AF = mybir.ActivationFunctionType
ALU = mybir.AluOpType

D = 1024
SEQ = 2048
DEPTH = 4
NCORES = 8
E = 32
DFF = 256
ALPHA = (2.0 * DEPTH) ** 0.25
LN_EPS = 1e-5
UNIT = 1024
TM = 256
TE = 512
WIN_FM = 2176
WIN_ALL = 2688
PI = math.pi

C_DECAY = 0
C_XI = 512
C_ZETA = 768
C_BD = 896
C_CAUS = 1152
C_ID = 1280
C_HM = 1408
C_CD = 1412
C_INVF = 1413
C_SGN = 1414
C_ONES = 1416
NCONST = 1544

V_BADA = 0
V_LN1G = 192
V_LN1B = 224
V_LN2G = 256
V_LN2B = 288
V_BGATE = 320
V_CONVW = 416
NVEC = 512


def _consts():
    c = np.zeros((128, NCONST), np.float64)
    gam = 1.0 - 2.0 ** (-5.0 - np.arange(4))
    lg = np.log(gam)
    idx = np.arange(128)
    sc = 32 ** -0.5
    for h in range(4):
        diff = idx[None, :] - idx[:, None]
        dm = np.where(diff >= 0, np.exp(lg[h] * np.maximum(diff, 0)), 0.0) * sc
        c[:, C_DECAY + h * 128:C_DECAY + (h + 1) * 128] = dm
    hd = idx // 32
    xi = np.exp(lg[hd][:, None] * (idx[None, :] + 1.0))
    c[:, C_XI:C_XI + 128] = xi
    c[:, C_XI + 128:C_XI + 256] = xi
    zeta = np.exp(lg[hd][None, :] * (127.0 - idx)[:, None]) * sc
    c[:, C_ZETA:C_ZETA + 128] = zeta
    hv = np.arange(256) // 64
    c[:, C_BD:C_BD + 256] = (hd[:, None] == hv[None, :]).astype(np.float64)
    c[:, C_CAUS:C_CAUS + 128] = (idx[:, None] <= idx[None, :]).astype(np.float64)
    c[:, C_ID:C_ID + 128] = np.eye(128)
    for h in range(4):
        c[:, C_HM + h] = (hd == h)
    c[:, C_CD] = np.exp(lg[hd] * 128.0)
    j = idx % 32
    invf = 10000.0 ** (-(2.0 * (j % 16)) / 32.0)
    c[:, C_INVF] = invf
    c[:, C_SGN] = np.where(j < 16, -1.0, 1.0)
    c[:, C_ONES:C_ONES + 128] = 1.0
    return c.astype(np.float32)


def _win_perm():
    o_q, o_k, o_v, o_g, o_gu, o_gv, o_cb, o_cc, o_cx, o_code = 0, 128, 256, 512, 768, 1024, 1280, 1536, 1792, 2048
    r = np.arange(128)
    j = r % 32
    perm = (r // 32) * 32 + (j + 16) % 32
    cols = np.concatenate([
        o_q + r, o_q + perm, o_k + r, o_k + perm,
        o_g + np.arange(256), o_gu + np.arange(256),
        o_cb + np.arange(256), o_cc + np.arange(256), o_cx + np.arange(256),
        o_code + np.arange(384),
        o_v + np.arange(256), o_gv + np.arange(256)])
    assert cols.shape[0] == WIN_ALL
    return cols


class Prog:
    def __init__(self, nc, es):
        self.nc = nc
        self.es = es
        self.lw = {}
        self.rd = {}
        self.engs = {}
        for name, eng in (("pe", nc.tensor), ("act", nc.scalar), ("dve", nc.vector), ("pool", nc.gpsimd), ("sp", nc.sync)):
            sem = es.enter_context(nc.semaphore("sem_" + name))
            self.engs[name] = dict(eng=eng, sem=sem, cnt=0, waited={}, name=name)
        self.dsems = {}
        self.nwait = 0
        self.ninst = 0

    def dsem(self, name):
        if name not in self.dsems:
            self.dsems[name] = dict(sem=self.es.enter_context(self.nc.semaphore("d_" + name)), tot=0)
        return self.dsems[name]

    FAST = {"f_a", "f_b", "f_c", "f_d", "f_e", "t_a", "t_b", "t_c", "gsil", "uT", "qr", "kr", "qxi", "sinT", "cosT", "xstage", "hT"}

    def _fast(self, k):
        return k in self.FAST or k.startswith(("XT", "cg", "cu", "cp"))

    def _deps(self, en, reads, writes):
        e = self.engs[en]
        deps = []
        for k in reads:
            t = self.lw.get(k)
            if t is not None:
                deps.append((t, k))
        for k in writes:
            t = self.lw.get(k)
            if t is not None:
                deps.append((t, k))
            for t in self.rd.get(k, {}).values():
                deps.append((t, k))
        for ((sem, val, owner), k) in deps:
            if owner == en and (en == "pe" or (en == "dve" and self._fast(k))):
                continue
            sid = id(sem)
            if e["waited"].get(sid, 0) >= val:
                continue
            e["eng"].wait_ge(sem, val)
            e["waited"][sid] = val
            self.nwait += 1

    def _mark(self, tok, en, reads, writes):
        for k in reads:
            self.rd.setdefault(k, {})[en] = tok
        for k in writes:
            self.lw[k] = tok
            self.rd[k] = {}

    def op(self, en, fn, reads=(), writes=()):
        e = self.engs[en]
        self._deps(en, reads, writes)
        inst = fn(e["eng"])
        e["cnt"] += 1
        inst.then_inc(e["sem"], 1)
        self.ninst += 1
        self._mark((e["sem"], e["cnt"], en), en, reads, writes)

    def dma(self, q, semname, items, reads=(), writes=()):
        e = self.engs[q]
        ds = self.dsem(semname)
        self._deps(q, reads, writes)
        if ds["tot"] > 0 and e["waited"].get(id(ds["sem"]), 0) < ds["tot"]:
            e["eng"].wait_ge(ds["sem"], ds["tot"])
            e["waited"][id(ds["sem"])] = ds["tot"]
        for (o, i) in items:
            inst = e["eng"].dma_start(out=o, in_=i)
            ds["tot"] += 16
            inst.then_inc(ds["sem"], 16)
            self.ninst += 1
        self._mark((ds["sem"], ds["tot"], "dma_" + semname), "dma_" + semname + q, reads, writes)

    def barrier(self):
        toks = [(e["sem"], e["cnt"]) for e in self.engs.values() if e["cnt"] > 0]
        toks += [(d["sem"], d["tot"]) for d in self.dsems.values() if d["tot"] > 0]
        for e in self.engs.values():
            for (sem, val) in toks:
                if sem is e["sem"]:
                    continue
                sid = id(sem)
                if e["waited"].get(sid, 0) >= val:
                    continue
                e["eng"].wait_ge(sem, val)
                e["waited"][sid] = val

    def finish(self, q):
        e = self.engs[q]
        for d in self.dsems.values():
            if d["tot"] > 0:
                e["eng"].wait_ge(d["sem"], d["tot"])
        for o in self.engs.values():
            if o is not e and o["cnt"] > 0:
                e["eng"].wait_ge(o["sem"], o["cnt"])


def build(NB=4, NL=DEPTH, dbg=False):
    nc = bass.Bass("TRN2", target_bir_lowering=False)
    es = contextlib.ExitStack()
    P = Prog(nc, es)

    def din(name, shape, dt=F32):
        return nc.dram_tensor(name, list(shape), dt, kind="ExternalInput").ap()

    x_d = din("x", [NB, SEQ, D])
    cT_d = din("cT", [128, 8, NB])
    pos_d = din("pos", [NB, SEQ], I32)
    wada_d = din("w_ada", [DEPTH, D, 6 * D])
    win_d = din("w_in", [DEPTH, D, WIN_ALL])
    vecs_d = din("vecs", [NVEC, 128])
    const_d = din("consts", [128, NCONST])
    lng_d = din("gmlp_ln_g", [DEPTH, 256])
    lnb_d = din("gmlp_ln_b", [DEPTH, 256])
    wsp_d = din("w_spatial", [DEPTH, 4, 128, 128])
    bsp_d = din("b_spatial", [DEPTH, 4, 128])
    wbr_d = din("w_branch", [DEPTH, 768, D])
    wg_d = din("w_gate_up", [DEPTH, 384, D])
    wout_d = din("w_out", [DEPTH, D, D])
    wr_d = din("w_router", [DEPTH, D, E])
    br_d = din("b_router", [DEPTH, E])
    wgu_d = din("w_gu", [DEPTH, E, D, 2 * DFF])
    bgu_d = din("b_gu", [DEPTH, E, 2 * DFF])
    wd_d = din("w_down", [DEPTH, E, DFF, D])
    bd_d = din("b_down", [DEPTH, E, D])
    ind_d = din("ind", [1, 2 * DFF])
    out_d = nc.dram_tensor("out", [NB, SEQ, D], F32, kind="ExternalOutput").ap()

    def sb(name, shape, dt=F32):
        return es.enter_context(nc.sbuf_tensor(name, list(shape), dt))

    XT = sb("XT", [128, 8, UNIT])
    h2T = sb("h2T", [128, 8, UNIT], BF16)
    CT = sb("CT", [128, NCONST])
    identb = sb("identb", [128, 128], BF16)
    vecT = sb("vecT", [128, NVEC])
    adaT = sb("adaT", [128, NL, 48, NB])
    dsc = sb("dsc", [128, NL, NB, 4, 8])
    cosT = sb("cosT", [128, UNIT])
    sinT = sb("sinT", [128, UNIT])
    wgt = sb("wgt", [128, UNIT // 128, E])
    state = sb("state", [128, NL, 256])
    state_bf = sb("state_bf", [128, NL, 256], BF16)
    zhalo = sb("zhalo", [128, NL, 2, 2])
    lngb = sb("lngb", [128, 2, 256])
    WmT = sb("WmT", [128, 4, 128], BF16)
    bsp_row = sb("bsp_row", [1, 512], BF16)
    brt_row = sb("brt_row", [1, E], BF16)
    Wr = sb("Wr", [128, 8, E], BF16)
    bdn = sb("bdn", [E, D], BF16)
    ones_bf = sb("ones_bf", [2, 128], BF16)
    small = sb("small", [128, 64])
    Win = sb("Win", [128, 8, WIN_ALL], BF16)
    Wbr = sb("Wbr", [128, 6, D], BF16)
    Wg = sb("Wg", [128, 3, D], BF16)
    Wout = sb("Wout", [128, 8, D], BF16)
    ARENA = 46 * 1024 // 4
    arena = sb("arena", [128, ARENA])
    apos = [0]

    def carve(n_elems, dt=F32):
        nb = n_elems * (4 if dt in (F32, I32) else 2)
        nw = (nb + 3) // 4
        a = apos[0]
        apos[0] += nw
        assert apos[0] <= ARENA, ("arena overflow", apos[0], ARENA)
        v = arena[:, a:a + nw]
        return v if dt == F32 else v.bitcast(dt)

    apos[0] = 0
    hT = carve(8 * TM, BF16).rearrange("p (k t) -> p k t", k=8)
    rskT = carve(6 * TM, BF16).rearrange("p (k t) -> p k t", k=6)
    codeT = carve(3 * TM, BF16).rearrange("p (k t) -> p k t", k=3)
    qr = carve(TM, BF16)
    kr = carve(TM, BF16)
    qxi = carve(TM, BF16)
    kmask = carve(4 * TM, BF16).rearrange("p (k t) -> p k t", k=4)
    f_a = carve(2 * TM)
    f_b = carve(2 * TM)
    f_c = carve(2 * TM)
    f_d = carve(2 * TM)
    f_e = carve(2 * TM)
    gsil = carve(2 * TM).rearrange("p (k t) -> p k t", k=2)
    uT = carve(2 * TM).rearrange("p (k t) -> p k t", k=2)
    zbuf = carve(2 * (TM + 2)).rearrange("p (k t) -> p k t", k=2)
    vtok = carve(256, BF16)
    vln = carve(256, BF16)
    Sd = carve(512, BF16)
    kz = carve(128, BF16)
    t_a = carve(256)
    t_b = carve(256)
    t_c = carve(256)
    xstage = carve(1024)
    mix_end = apos[0]
    apos[0] = 0
    Wgu = [carve(8 * 512, BF16).rearrange("p (k f) -> p k f", k=8) for _ in range(2)]
    Wd = [carve(2 * D, BF16).rearrange("p (k f) -> p k f", k=2) for _ in range(2)]
    bgu = [carve(512, BF16) for _ in range(2)]
    NCH = 2
    cg = [carve(256) for _ in range(NCH)]
    csig = [carve(256) for _ in range(NCH)]
    cu = [carve(256) for _ in range(NCH)]
    cp_ = [carve(256) for _ in range(NCH)]
    cact = [carve(256, BF16) for _ in range(4)]
    actT = [carve(2 * TE, BF16).rearrange("p (k t) -> p k t", k=2) for _ in range(2)]
    wgtT = carve(TE)
    rt_a = carve(E)
    rt_b = carve(E)
    top8 = carve(8)
    moe_end = apos[0]

    banks = [es.enter_context(nc.psum_tensor("ps%d" % i, [128, 512], F32)) for i in range(8)]
    bk = [0]

    def psn():
        i = bk[0] % 8
        bk[0] += 1
        return banks[i], "ps%d" % i

    op, dma = P.op, P.dma
    ident = CT[:, C_ID:C_ID + 128]
    ones_f = CT[:, C_ONES:C_ONES + 128]

    dma("sp", "const", [(CT[:], const_d)], writes=["CT"])
    op("dve", lambda v: v.tensor_copy(out=identb[:], in_=ident), reads=["CT"], writes=["identb"])
    op("dve", lambda v: v.memset(ones_bf[:], 1.0), writes=["ones_bf"])
    for r in range(NVEC // 128):
        dma("sp", "setup", [(f_a[:, 0:128], vecs_d[r * 128:(r + 1) * 128, :])], writes=["f_a"])
        pb, pk = psn()
        op("pe", lambda t: t.transpose(out=pb[:, 0:128], in_=f_a[:, 0:128], identity=ident), reads=["f_a", "CT"], writes=[pk])
        op("dve", lambda v: v.tensor_copy(out=vecT[:, r * 128:(r + 1) * 128], in_=pb[:, 0:128]), reads=[pk], writes=["vecT"])
    cact_t = t_a[:, 0:8 * NB].rearrange("p (k b) -> p k b", k=8)
    csg = t_b[:, 0:8 * NB].rearrange("p (k b) -> p k b", k=8)
    cbf = vtok[:, 0:8 * NB].rearrange("p (k b) -> p k b", k=8)
    dma("sp", "setup", [(cact_t, cT_d)], writes=["t_a"])
    op("act", lambda a: a.activation(out=csg, in_=cact_t, func=AF.Sigmoid), reads=["t_a"], writes=["t_b"])
    op("dve", lambda v: v.tensor_tensor(out=cbf, in0=cact_t, in1=csg, op=ALU.mult), reads=["t_a", "t_b"], writes=["vtok"])
    WA = Win[:, :, 0:1536]
    for l in range(NL):
        for pc in range(4):
            dma("pool", "wada", [(WA, wada_d[l, :, pc * 1536:(pc + 1) * 1536].rearrange("(k p) n -> p k n", p=128))], writes=["Win"])
            for jj in range(12):
                j = pc * 12 + jj
                pb, pk = psn()
                for kc in range(8):
                    op("pe", lambda t: t.matmul(pb[:, 0:NB], lhsT=WA[:, kc, jj * 128:(jj + 1) * 128], rhs=cbf[:, kc, :],
                                                start=(kc == 0), stop=(kc == 7)), reads=["Win", "vtok"], writes=[pk])
                op("dve", lambda v: v.tensor_scalar(out=adaT[:, l, j, :], in0=pb[:, 0:NB], scalar1=vecT[:, V_BADA + l * 48 + j:V_BADA + l * 48 + j + 1],
                                                    scalar2=None, op0=ALU.add), reads=[pk, "vecT"], writes=["adaT"])
    for l in range(NL):
        for b in range(NB):
            op("dve", lambda v: v.tensor_scalar(out=dsc[:, l, b, 0, :], in0=adaT[:, l, 8:16, b], scalar1=1.0, scalar2=None, op0=ALU.add), reads=["adaT"], writes=["dsc"])
            op("dve", lambda v: v.tensor_scalar(out=dsc[:, l, b, 1, :], in0=adaT[:, l, 16:24, b], scalar1=1.0, scalar2=1.0 / ALPHA, op0=ALU.add, op1=ALU.mult), reads=["adaT"], writes=["dsc"])
            op("dve", lambda v: v.tensor_scalar(out=dsc[:, l, b, 2, :], in0=adaT[:, l, 32:40, b], scalar1=1.0, scalar2=None, op0=ALU.add), reads=["adaT"], writes=["dsc"])
            op("dve", lambda v: v.tensor_scalar(out=dsc[:, l, b, 3, :], in0=adaT[:, l, 40:48, b], scalar1=1.0, scalar2=1.0 / ALPHA, op0=ALU.add, op1=ALU.mult), reads=["adaT"], writes=["dsc"])

    def col(ap2d, j):
        return ap2d[:, j:j + 1]

    def gelu_tanh(src_ps, srck, dst, dstk, n, tmp1, tmp1k, tmp2, tmp2k):
        op("act", lambda a: a.activation(out=tmp1[:, 0:n], in_=src_ps, func=AF.Square), reads=[srck], writes=[tmp1k])
        op("dve", lambda v: v.tensor_scalar(out=tmp1[:, 0:n], in0=tmp1[:, 0:n], scalar1=0.044715, scalar2=1.0, op0=ALU.mult, op1=ALU.add), reads=[tmp1k], writes=[tmp1k])
        op("dve", lambda v: v.tensor_tensor(out=tmp1[:, 0:n], in0=src_ps, in1=tmp1[:, 0:n], op=ALU.mult), reads=[srck, tmp1k], writes=[tmp1k])
        op("act", lambda a: a.activation(out=tmp2[:, 0:n], in_=tmp1[:, 0:n], func=AF.Sigmoid, scale=1.5957691216057308), reads=[tmp1k], writes=[tmp2k])
        op("dve", lambda v: v.tensor_tensor(out=dst, in0=src_ps, in1=tmp2[:, 0:n], op=ALU.mult), reads=[srck, tmp2k], writes=[dstk])

    def rstd_from_var(var_ap, out_ap, eps, rk, wk):
        op("dve", lambda v: v.tensor_scalar(out=out_ap, in0=var_ap, scalar1=float(eps), scalar2=None, op0=ALU.add), reads=rk, writes=wk)
        op("act", lambda a: a.activation(out=out_ap, in_=out_ap, func=AF.Sqrt), reads=wk, writes=wk)
        op("dve", lambda v: v.reciprocal(out=out_ap, in_=out_ap), reads=wk, writes=wk)

    def load_mixer_weights(l):
        wl = lambda ap: ap.rearrange("(k p) n -> p k n", p=128)
        dma("pool", "wmix", [(Win[:, :, 0:1344], wl(win_d[l, :, 0:1344])), (Win[:, :, 1344:2688], wl(win_d[l, :, 1344:2688])), (Wbr[:], wl(wbr_d[l])), (Wg[:], wl(wg_d[l])), (Wout[:], wl(wout_d[l]))],
            writes=["Win", "Wbr", "Wg", "Wout"])
    first_mixer = [True]

    for b in range(NB):
        for hf in range(SEQ // UNIT):
            tok0 = hf * UNIT
            P.barrier()
            posi = xstage.bitcast(I32)
            dma("sp", "setup", [(posi, pos_d[b:b + 1, tok0:tok0 + UNIT].partition_broadcast(128))], writes=["xstage"])
            tmpI = hT.rearrange("p k t -> p (k t)").bitcast(I32)
            op("dve", lambda v: v.tensor_copy(out=sinT[:], in_=posi), reads=["xstage"], writes=["sinT"])
            op("dve", lambda v: v.tensor_scalar(out=sinT[:], in0=sinT[:], scalar1=CT[:, C_INVF:C_INVF + 1], scalar2=None, op0=ALU.mult), reads=["sinT", "CT"], writes=["sinT"])
            op("dve", lambda v: v.tensor_scalar(out=cosT[:], in0=sinT[:], scalar1=PI / 2, scalar2=None, op0=ALU.add), reads=["sinT"], writes=["cosT"])
            for (tab, tk, sgn) in ((sinT, "sinT", True), (cosT, "cosT", False)):
                ki = tmpI
                kf = xstage
                op("dve", lambda v: v.tensor_scalar(out=ki, in0=tab[:], scalar1=1.0 / (2 * PI), scalar2=None, op0=ALU.mult), reads=[tk], writes=["hT"])
                op("dve", lambda v: v.tensor_copy(out=kf, in_=ki), reads=["hT", "sinT"], writes=["xstage"])
                op("dve", lambda v: v.scalar_tensor_tensor(out=tab[:], in0=kf, scalar=-2 * PI, in1=tab[:], op0=ALU.mult, op1=ALU.add), reads=["xstage", tk], writes=[tk])
                op("dve", lambda v: v.tensor_scalar(out=kf, in0=tab[:], scalar1=PI, scalar2=2 * PI, op0=ALU.is_gt, op1=ALU.mult), reads=[tk], writes=["xstage"])
                op("dve", lambda v: v.tensor_tensor(out=tab[:], in0=tab[:], in1=kf, op=ALU.subtract), reads=[tk, "xstage"], writes=[tk])
                op("dve", lambda v: v.tensor_scalar(out=kf, in0=tab[:], scalar1=-PI, scalar2=2 * PI, op0=ALU.is_lt, op1=ALU.mult), reads=[tk], writes=["xstage"])
                op("dve", lambda v: v.tensor_tensor(out=tab[:], in0=tab[:], in1=kf, op=ALU.add), reads=[tk, "xstage"], writes=[tk])
                op("dve", lambda v: v.tensor_scalar(out=tab[:], in0=tab[:], scalar1=-PI, scalar2=PI, op0=ALU.max, op1=ALU.min), reads=[tk], writes=[tk])
                op("act", lambda a: a.activation(out=tab[:], in_=tab[:], func=AF.Sin), reads=[tk], writes=[tk])
                if sgn:
                    op("dve", lambda v: v.tensor_scalar(out=tab[:], in0=tab[:], scalar1=CT[:, C_SGN:C_SGN + 1], scalar2=None, op0=ALU.mult), reads=[tk, "CT"], writes=[tk])
            for c in range(UNIT // 128):
                dma("sp", "xin", [(xstage, x_d[b, tok0 + c * 128:tok0 + (c + 1) * 128, :])], writes=["xstage"])
                for g4 in range(2):
                    pb, pk = psn()
                    for q in range(4):
                        kc = g4 * 4 + q
                        op("pe", lambda t: t.transpose(out=pb[:, q * 128:(q + 1) * 128], in_=xstage[:, kc * 128:(kc + 1) * 128], identity=ident),
                           reads=["xstage", "CT"], writes=[pk])
                    op("act", lambda a: a.activation(out=XT[:, g4 * 4:(g4 + 1) * 4, c * 128:(c + 1) * 128], in_=pb[:].rearrange("p (q t) -> p q t", q=4), func=AF.Identity),
                       reads=[pk], writes=["XT"])
            if hf == 0:
                op("dve", lambda v: v.memset(state[:], 0.0), writes=["state"])
                op("dve", lambda v: v.memset(state_bf[:], 0.0), writes=["state_bf"])
                op("dve", lambda v: v.memset(zhalo[:], 0.0), writes=["zhalo"])

            for l in range(NL):
                P.barrier()
                if first_mixer[0]:
                    load_mixer_weights(l)
                    first_mixer[0] = False
                last_unit = (b == NB - 1 and hf == SEQ // UNIT - 1)
                next_mixer_weights = (l + 1) if l + 1 < NL else (None if last_unit else 0)
                if True:
                    dma("sp", "setup", [(lngb[:, 0, :], lng_d[l:l + 1, :].partition_broadcast(128)), (lngb[:, 1, :], lnb_d[l:l + 1, :].partition_broadcast(128))], writes=["lngb"])
                    dma("pool", "wsm", [(bsp_row[:], bsp_d[l:l + 1].rearrange("o g c -> o (g c)")), (brt_row[:], br_d[l:l + 1, :]),
                                        (Wr[:], wr_d[l].rearrange("(k p) n -> p k n", p=128)), (bdn[:], bd_d[l])],
                        writes=["bsp_row", "brt_row", "Wr", "bdn"])
                    for g in range(4):
                        dma("sp", "setup", [(t_a[:, 0:128], wsp_d[l, g])], writes=["t_a"])
                        pb, pk = psn()
                        op("pe", lambda t: t.transpose(out=pb[:, 0:128], in_=t_a[:, 0:128], identity=ident), reads=["t_a", "CT"], writes=[pk])
                        op("dve", lambda v: v.tensor_tensor(out=WmT[:, g, :], in0=pb[:, 0:128], in1=CT[:, C_CAUS:C_CAUS + 128], op=ALU.mult), reads=[pk, "CT"], writes=["WmT"])
                A1 = dsc[:, l, b, 0, :]
                G1 = dsc[:, l, b, 1, :]
                A2 = dsc[:, l, b, 2, :]
                G2 = dsc[:, l, b, 3, :]
                for tj in range(UNIT // TM):
                    ts = slice(tj * TM, (tj + 1) * TM)
                    for kc in range(8):
                        op("act", lambda a: a.activation(out=hT[:, kc, :], in_=XT[:, kc, ts], func=AF.Identity, scale=col(A1, kc), bias=adaT[:, l, kc, b:b + 1]),
                           reads=["XT", "dsc", "adaT"], writes=["hT"])

                    def proj(c0, nch):
                        pb, pk = psn()
                        for q in range(nch):
                            for kc in range(8):
                                op("pe", lambda t: t.matmul(pb[:, q * TM:(q + 1) * TM], lhsT=Win[:, kc, (c0 + q) * 128:(c0 + q + 1) * 128], rhs=hT[:, kc, :],
                                                            start=(kc == 0), stop=(kc == 7)), reads=["Win", "hT"], writes=[pk])
                        return pb, pk
                    for (c0, dst, dk) in ((0, qr, "qr"), (2, kr, "kr")):
                        pb, pk = proj(c0, 2)
                        op("dve", lambda v: v.tensor_tensor(out=f_a[:, 0:TM], in0=pb[:, 0:TM], in1=cosT[:, ts], op=ALU.mult), reads=[pk, "cosT"], writes=["f_a"])
                        op("dve", lambda v: v.tensor_tensor(out=f_b[:, 0:TM], in0=pb[:, TM:2 * TM], in1=sinT[:, ts], op=ALU.mult), reads=[pk, "sinT"], writes=["f_b"])
                        op("dve", lambda v: v.tensor_tensor(out=dst, in0=f_a[:, 0:TM], in1=f_b[:, 0:TM], op=ALU.add), reads=["f_a", "f_b"], writes=[dk])
                    op("dve", lambda v: v.tensor_tensor(out=qxi, in0=qr, in1=CT[:, C_XI:C_XI + TM], op=ALU.mult), reads=["qr", "CT"], writes=["qxi"])
                    for h in range(4):
                        op("act", lambda a: a.activation(out=kmask[:, h, :], in_=kr, func=AF.Identity, scale=CT[:, C_HM + h:C_HM + h + 1]), reads=["kr", "CT"], writes=["kmask"])
                    pb, pk = proj(4, 2)
                    op("act", lambda a: a.activation(out=f_a, in_=pb[:], func=AF.Sigmoid), reads=[pk], writes=["f_a"])
                    op("dve", lambda v: v.tensor_tensor(out=gsil.rearrange("p k t -> p (k t)"), in0=pb[:], in1=f_a, op=ALU.mult), reads=[pk, "f_a"], writes=["gsil"])
                    pb, pk = proj(6, 2)
                    gelu_tanh(pb[:], pk, uT.rearrange("p k t -> p (k t)"), "uT", 2 * TM, f_b, "f_b", f_c, "f_c")
                    pcb, kcb = proj(8, 2)
                    pcc, kcc = proj(10, 2)
                    pcx, kcx = proj(12, 2)
                    op("act", lambda a: a.activation(out=f_a, in_=pcc[:], func=AF.Identity), reads=[kcc], writes=["f_a"])
                    for ch in range(2):
                        op("dve", lambda v: v.tensor_copy(out=zbuf[:, ch, 0:2], in_=zhalo[:, l, ch, :]), reads=["zhalo"], writes=["zbuf"])
                        op("dve", lambda v: v.tensor_tensor(out=zbuf[:, ch, 2:TM + 2], in0=pcx[:, ch * TM:(ch + 1) * TM], in1=f_a[:, ch * TM:(ch + 1) * TM], op=ALU.mult),
                           reads=[kcx, "f_a"], writes=["zbuf"])
                        op("dve", lambda v: v.tensor_copy(out=zhalo[:, l, ch, :], in_=zbuf[:, ch, TM:TM + 2]), reads=["zbuf"], writes=["zhalo"])
                        w0 = vecT[:, V_CONVW + (l * 3 + 0) * 2 + ch:V_CONVW + (l * 3 + 0) * 2 + ch + 1]
                        w1 = vecT[:, V_CONVW + (l * 3 + 1) * 2 + ch:V_CONVW + (l * 3 + 1) * 2 + ch + 1]
                        w2 = vecT[:, V_CONVW + (l * 3 + 2) * 2 + ch:V_CONVW + (l * 3 + 2) * 2 + ch + 1]
                        acc = f_d[:, ch * TM:(ch + 1) * TM]
                        op("dve", lambda v: v.tensor_scalar(out=acc, in0=zbuf[:, ch, 2:TM + 2], scalar1=w2, scalar2=None, op0=ALU.mult), reads=["zbuf", "vecT"], writes=["f_d"])
                        op("dve", lambda v: v.scalar_tensor_tensor(out=acc, in0=zbuf[:, ch, 1:TM + 1], scalar=w1, in1=acc, op0=ALU.mult, op1=ALU.add), reads=["zbuf", "vecT", "f_d"], writes=["f_d"])
                        op("dve", lambda v: v.scalar_tensor_tensor(out=acc, in0=zbuf[:, ch, 0:TM], scalar=w0, in1=acc, op0=ALU.mult, op1=ALU.add), reads=["zbuf", "vecT", "f_d"], writes=["f_d"])
                    op("dve", lambda v: v.tensor_tensor(out=rskT[:, 4:6, :].rearrange("p k t -> p (k t)"), in0=pcb[:], in1=f_d, op=ALU.mult), reads=[kcb, "f_d"], writes=["rskT"])
                    pb, pk = proj(14, 2)
                    op("act", lambda a: a.activation(out=codeT[:, 0:2, :].rearrange("p k t -> p (k t)"), in_=pb[:], func=AF.Identity), reads=[pk], writes=["codeT"])
                    pb, pk = proj(16, 1)
                    op("act", lambda a: a.activation(out=codeT[:, 2, :], in_=pb[:, 0:TM], func=AF.Identity), reads=[pk], writes=["codeT"])
                    for cc_ in range(TM // 128):
                        cs = slice(cc_ * 128, (cc_ + 1) * 128)
                        ptm, ktm = psn()
                        for kc in range(8):
                            op("pe", lambda t: t.matmul(ptm[:], lhsT=hT[:, kc, cs], rhs=Win[:, kc, WIN_FM:WIN_ALL], start=(kc == 0), stop=(kc == 7)), reads=["hT", "Win"], writes=[ktm])
                        op("act", lambda a: a.activation(out=vtok, in_=ptm[:, 0:256], func=AF.Identity), reads=[ktm], writes=["vtok"])
                        psc, ksc = psn()
                        for h in range(4):
                            op("pe", lambda t: t.matmul(psc[:, h * 128:(h + 1) * 128], lhsT=kmask[:, h, cs], rhs=qr[:, cs], start=True, stop=True), reads=["kmask", "qr"], writes=[ksc])
                        op("dve", lambda v: v.tensor_tensor(out=Sd, in0=psc[:], in1=CT[:, C_DECAY:C_DECAY + 512], op=ALU.mult), reads=[ksc, "CT"], writes=["Sd"])
                        po, ko = psn()
                        op("pe", lambda t: t.matmul(po[:, 0:256], lhsT=qxi[:, cs], rhs=state_bf[:, l, :], start=True, stop=False), reads=["qxi", "state_bf"], writes=[ko])
                        for h in range(4):
                            op("pe", lambda t: t.matmul(po[:, h * 64:(h + 1) * 64], lhsT=Sd[:, h * 128:(h + 1) * 128], rhs=vtok[:, h * 64:(h + 1) * 64], start=False, stop=(h == 3)),
                               reads=["Sd", "vtok"], writes=[ko])
                        gelu_tanh(ptm[:, 256:512], ktm, t_a, "t_a", 256, t_b, "t_b", t_c, "t_c")
                        op("dve", lambda v: v.bn_stats(out=small[:, 0:6], in_=t_a), reads=["t_a"], writes=["small"])
                        op("dve", lambda v: v.bn_aggr(out=small[:, 8:10], in_=small[:, 0:6]), reads=["small"], writes=["small"])
                        rstd_from_var(small[:, 9:10], small[:, 10:11], LN_EPS, ["small"], ["small"])
                        op("dve", lambda v: v.tensor_scalar(out=t_a, in0=t_a, scalar1=small[:, 8:9], scalar2=small[:, 10:11], op0=ALU.subtract, op1=ALU.mult), reads=["t_a", "small"], writes=["t_a"])
                        op("dve", lambda v: v.tensor_tensor(out=t_a, in0=t_a, in1=lngb[:, 0, :], op=ALU.mult), reads=["t_a", "lngb"], writes=["t_a"])
                        op("dve", lambda v: v.tensor_tensor(out=vln, in0=t_a, in1=lngb[:, 1, :], op=ALU.add), reads=["t_a", "lngb"], writes=["vln"])
                        pz, kzk = psn()
                        for g in range(4):
                            op("pe", lambda t: t.matmul(pz[:, g * 64:(g + 1) * 64], lhsT=WmT[:, g, :], rhs=vln[:, g * 64:(g + 1) * 64], start=True, stop=False), reads=["WmT", "vln"], writes=[kzk])
                            op("pe", lambda t: t.matmul(pz[:, g * 64:(g + 1) * 64], lhsT=bsp_row[0:1, g * 128:(g + 1) * 128], rhs=ones_bf[0:1, 0:64], start=False, stop=True),
                               reads=["bsp_row", "ones_bf"], writes=[kzk])
                        op("act", lambda a: a.activation(out=t_b, in_=pz[:, 0:256], func=AF.Identity), reads=[kzk], writes=["t_b"])
                        ptr, ktr = psn()
                        for q in range(2):
                            op("pe", lambda t: t.transpose(out=ptr[:, q * 128:(q + 1) * 128], in_=t_b[:, q * 128:(q + 1) * 128], identity=ident), reads=["t_b", "CT"], writes=[ktr])
                        op("dve", lambda v: v.tensor_tensor(out=rskT[:, 2:4, cs], in0=ptr[:, 0:256].rearrange("p (q t) -> p q t", q=2), in1=uT[:, :, cs], op=ALU.mult), reads=[ktr, "uT"], writes=["rskT"])
                        for h in range(4):
                            op("dve", lambda v: v.bn_stats(out=small[:, 16 + h * 6:22 + h * 6], in_=po[:, h * 64:(h + 1) * 64]), reads=[ko], writes=["small"])
                        for h in range(4):
                            op("dve", lambda v: v.bn_aggr(out=small[:, 40 + 2 * h:42 + 2 * h], in_=small[:, 16 + h * 6:22 + h * 6]), reads=["small"], writes=["small"])
                        sm3 = small[:, 40:48].rearrange("p (h t) -> p h t", t=2)
                        rstd_from_var(sm3[:, :, 1], small[:, 48:52], LN_EPS, ["small"], ["small"])
                        for h in range(4):
                            op("dve", lambda v: v.tensor_scalar(out=t_c[:, h * 64:(h + 1) * 64], in0=po[:, h * 64:(h + 1) * 64], scalar1=small[:, 40 + 2 * h:41 + 2 * h],
                                                                scalar2=small[:, 48 + h:49 + h], op0=ALU.subtract, op1=ALU.mult), reads=[ko, "small"], writes=["t_c"])
                        ptr, ktr = psn()
                        for q in range(2):
                            op("pe", lambda t: t.transpose(out=ptr[:, q * 128:(q + 1) * 128], in_=t_c[:, q * 128:(q + 1) * 128], identity=ident), reads=["t_c", "CT"], writes=[ktr])
                        op("dve", lambda v: v.tensor_tensor(out=rskT[:, 0:2, cs], in0=ptr[:, 0:256].rearrange("p (q t) -> p q t", q=2), in1=gsil[:, :, cs], op=ALU.mult), reads=[ktr, "gsil"], writes=["rskT"])
                        pkt, kkt = psn()
                        pkt_b = pkt[:].bitcast(BF16)
                        op("pe", lambda t: t.transpose(out=pkt_b[:, 0:128], in_=kr[:, cs], identity=identb[:]), reads=["kr", "identb"], writes=[kkt])
                        op("dve", lambda v: v.tensor_tensor(out=kz, in0=pkt_b[:, 0:128], in1=CT[:, C_ZETA:C_ZETA + 128], op=ALU.mult), reads=[kkt, "CT"], writes=["kz"])
                        pkv, kkv = psn()
                        op("pe", lambda t: t.matmul(pkv[:, 0:256], lhsT=kz, rhs=vtok, start=True, stop=True), reads=["kz", "vtok"], writes=[kkv])
                        op("dve", lambda v: v.tensor_tensor(out=t_b, in0=pkv[:, 0:256], in1=CT[:, C_BD:C_BD + 256], op=ALU.mult), reads=[kkv, "CT"], writes=["t_b"])
                        op("dve", lambda v: v.scalar_tensor_tensor(out=state[:, l, :], in0=state[:, l, :], scalar=CT[:, C_CD:C_CD + 1], in1=t_b, op0=ALU.mult, op1=ALU.add),
                           reads=["state", "CT", "t_b"], writes=["state"])
                        op("act", lambda a: a.activation(out=state_bf[:, l, :], in_=state[:, l, :], func=AF.Identity), reads=["state"], writes=["state_bf"])
                    mergedT = hT
                    for dc in range(8):
                        dsl = slice(dc * 128, (dc + 1) * 128)
                        for i in range(3):
                            py, ky = psn()
                            for kk in range(2):
                                op("pe", lambda t: t.matmul(py[:, 0:TM], lhsT=Wbr[:, i * 2 + kk, dsl], rhs=rskT[:, i * 2 + kk, :], start=(kk == 0), stop=(kk == 1)), reads=["Wbr", "rskT"], writes=[ky])
                            op("pe", lambda t: t.matmul(py[:, TM:2 * TM], lhsT=Wg[:, i, dsl], rhs=codeT[:, i, :], start=True, stop=True), reads=["Wg", "codeT"], writes=[ky])
                            bg = vecT[:, V_BGATE + (l * 3 + i) * 8 + dc:V_BGATE + (l * 3 + i) * 8 + dc + 1]
                            sgb = (f_a, f_d, f_e)[i]
                            sgk = ("f_a", "f_d", "f_e")[i]
                            op("act", lambda a: a.activation(out=sgb[:, 0:TM], in_=py[:, TM:2 * TM], func=AF.Sigmoid, bias=bg), reads=[ky, "vecT"], writes=[sgk])
                            if i == 0:
                                op("dve", lambda v: v.tensor_tensor(out=f_b[:, 0:TM], in0=py[:, 0:TM], in1=sgb[:, 0:TM], op=ALU.mult), reads=[ky, sgk], writes=["f_b"])
                            else:
                                op("dve", lambda v: v.tensor_tensor(out=f_c[:, 0:TM], in0=py[:, 0:TM], in1=sgb[:, 0:TM], op=ALU.mult), reads=[ky, sgk], writes=["f_c"])
                                if i == 1:
                                    op("dve", lambda v: v.tensor_tensor(out=f_b[:, 0:TM], in0=f_b[:, 0:TM], in1=f_c[:, 0:TM], op=ALU.add), reads=["f_b", "f_c"], writes=["f_b"])
                                else:
                                    op("dve", lambda v: v.tensor_tensor(out=mergedT[:, dc, :], in0=f_b[:, 0:TM], in1=f_c[:, 0:TM], op=ALU.add), reads=["f_b", "f_c"], writes=["hT"])
                    for dc in range(8):
                        pm, km = psn()
                        for kc in range(8):
                            op("pe", lambda t: t.matmul(pm[:, 0:TM], lhsT=Wout[:, kc, dc * 128:(dc + 1) * 128], rhs=mergedT[:, kc, :], start=(kc == 0), stop=(kc == 7)), reads=["Wout", "hT"], writes=[km])
                        op("dve", lambda v: v.scalar_tensor_tensor(out=XT[:, dc, ts], in0=pm[:, 0:TM], scalar=col(G1, dc), in1=XT[:, dc, ts], op0=ALU.mult, op1=ALU.add),
                           reads=[km, "dsc", "XT"], writes=["XT"])
                    layer_norm_tile = None

                    def ln_tile(tsl, n, gvrow, bvrow, post):
                        ps1, k1 = psn()
                        for kc in range(8):
                            op("pe", lambda t: t.matmul(ps1[:, 0:n], lhsT=ones_f, rhs=XT[:, kc, tsl], start=(kc == 0), stop=(kc == 7)), reads=["CT", "XT"], writes=[k1])
                        ps2, k2 = psn()
                        for kc in range(8):
                            sq = (f_a, f_b)[kc % 2]
                            sqk = ("f_a", "f_b")[kc % 2]
                            op("act", lambda a: a.activation(out=sq[:, 0:n], in_=XT[:, kc, tsl], func=AF.Square), reads=["XT"], writes=[sqk])
                            op("pe", lambda t: t.matmul(ps2[:, 0:n], lhsT=ones_f, rhs=sq[:, 0:n], start=(kc == 0), stop=(kc == 7)), reads=["CT", sqk], writes=[k2])
                        mean = f_c[:, 0:n]
                        rstd = f_d[:, 0:n]
                        nmr = f_e[:, 0:n]
                        op("act", lambda a: a.activation(out=mean, in_=ps1[:, 0:n], func=AF.Identity, scale=1.0 / D), reads=[k1], writes=["f_c"])
                        op("dve", lambda v: v.tensor_tensor(out=nmr, in0=mean, in1=mean, op=ALU.mult), reads=["f_c"], writes=["f_e"])
                        op("dve", lambda v: v.scalar_tensor_tensor(out=rstd, in0=ps2[:, 0:n], scalar=1.0 / D, in1=nmr, op0=ALU.mult, op1=ALU.subtract), reads=[k2, "f_e"], writes=["f_d"])
                        rstd_from_var(rstd, rstd, LN_EPS / (ALPHA * ALPHA), ["f_d"], ["f_d"])
                        op("dve", lambda v: v.scalar_tensor_tensor(out=nmr, in0=mean, scalar=-1.0, in1=rstd, op0=ALU.mult, op1=ALU.mult), reads=["f_c", "f_d"], writes=["f_e"])
                        for kc in range(8):
                            xn = (f_a, f_b)[kc % 2][:, 0:n]
                            xk = ("f_a", "f_b")[kc % 2]
                            op("dve", lambda v: v.tensor_tensor(out=xn, in0=XT[:, kc, tsl], in1=rstd, op=ALU.mult), reads=["XT", "f_d"], writes=[xk])
                            op("dve", lambda v: v.tensor_tensor(out=xn, in0=xn, in1=nmr, op=ALU.add), reads=[xk, "f_e"], writes=[xk])
                            op("act", lambda a: a.activation(out=XT[:, kc, tsl], in_=xn, func=AF.Identity, scale=vecT[:, gvrow + kc:gvrow + kc + 1], bias=vecT[:, bvrow + kc:bvrow + kc + 1]),
                               reads=[xk, "vecT"], writes=["XT"])
                            if post is not None:
                                post(kc)
                    ln_tile(ts, TM, V_LN1G + l * 8, V_LN1B + l * 8,
                            lambda kc: op("act", lambda a: a.activation(out=h2T[:, kc, ts], in_=XT[:, kc, ts], func=AF.Identity, scale=col(A2, kc), bias=adaT[:, l, 24 + kc, b:b + 1]),
                                          reads=["XT", "dsc", "adaT"], writes=["h2T"]))

                P.barrier()
                NS = UNIT // 128
                for s in range(NS):
                    ss = slice(s * 128, (s + 1) * 128)
                    pr, kr_ = psn()
                    for kc in range(8):
                        op("pe", lambda t: t.matmul(pr[:, 0:E], lhsT=h2T[:, kc, ss], rhs=Wr[:, kc, :], start=(kc == 0), stop=False), reads=["h2T", "Wr"], writes=[kr_])
                    op("pe", lambda t: t.matmul(pr[:, 0:E], lhsT=ones_bf[0:1, :], rhs=brt_row[0:1, :], start=False, stop=True), reads=["ones_bf", "brt_row"], writes=[kr_])
                    op("dve", lambda v: v.tensor_copy(out=rt_a, in_=pr[:, 0:E]), reads=[kr_], writes=["rt_a"])
                    op("dve", lambda v: v.max(out=top8, in_=rt_a), reads=["rt_a"], writes=["top8"])
                    op("dve", lambda v: v.tensor_scalar(out=small[:, 56:57], in0=top8[:, 0:1], scalar1=-1.0, scalar2=None, op0=ALU.mult), reads=["top8"], writes=["small"])
                    op("act", lambda a: a.activation(out=rt_b, in_=rt_a, func=AF.Exp, bias=small[:, 56:57]), reads=["rt_a", "small"], writes=["rt_b"])
                    op("dve", lambda v: v.scalar_tensor_tensor(out=rt_b, in0=rt_a, scalar=top8[:, 3:4], in1=rt_b, op0=ALU.is_ge, op1=ALU.mult), reads=["rt_a", "top8", "rt_b"], writes=["rt_b"])
                    op("dve", lambda v: v.reduce_sum(out=small[:, 57:58], in_=rt_b, axis=mybir.AxisListType.X), reads=["rt_b"], writes=["small"])
                    op("dve", lambda v: v.reciprocal(out=small[:, 58:59], in_=small[:, 57:58]), reads=["small"], writes=["small"])
                    op("dve", lambda v: v.tensor_scalar(out=wgt[:, s, :], in0=rt_b, scalar1=small[:, 58:59], scalar2=None, op0=ALU.mult), reads=["rt_b", "small"], writes=["wgt"])
                for tt in range(UNIT // TE):
                    for s4 in range(4):
                        s = tt * 4 + s4
                        pw, kw = psn()
                        op("pe", lambda t: t.transpose(out=pw[0:E, 0:128], in_=wgt[:, s, :], identity=ident), reads=["wgt", "CT"], writes=[kw])
                        op("act", lambda a: a.activation(out=wgtT.bitcast(BF16)[0:E, s4 * 128:(s4 + 1) * 128], in_=pw[0:E, 0:128], func=AF.Identity), reads=[kw], writes=["wgtT"])
                    for dc in range(8):
                        pbd, kbd = psn()
                        op("pe", lambda t: t.matmul(pbd[:], lhsT=bdn[:, dc * 128:(dc + 1) * 128], rhs=wgtT.bitcast(BF16)[0:E, 0:TE], start=True, stop=True), reads=["bdn", "wgtT"], writes=[kbd])
                        xk = "XT%d_%d" % (tt, dc)
                        op("dve", lambda v: v.scalar_tensor_tensor(out=XT[:, dc, tt * TE:(tt + 1) * TE], in0=pbd[:], scalar=col(G2, dc), in1=XT[:, dc, tt * TE:(tt + 1) * TE], op0=ALU.mult, op1=ALU.add),
                           reads=[kbd, "dsc", "XT"], writes=["XT", xk])

                def load_gu(e):
                    sl = e % 2
                    dma("pool", "wexp%d" % sl,
                        [(Wgu[sl], wgu_d[l, e].rearrange("(k p) n -> p k n", p=128)),
                         (bgu[sl][0:1, :], bgu_d[l, e:e + 1, :])],
                        writes=["Wgu%d" % sl, "bgu%d" % sl])

                def load_d(e):
                    sl = e % 2
                    dma("pool", "wexd%d" % sl,
                        [(Wd[sl], wd_d[l, e].rearrange("(k p) n -> p k n", p=128))],
                        writes=["Wd%d" % sl])

                dma("pool", "wsm", [(bgu[0][1:2, :], ind_d), (bgu[1][1:2, :], ind_d)], writes=["bgu0", "bgu1"])
                NT = UNIT // TE
                units = [(e, tt) for e in range(E) for tt in range(NT)]
                nH = len(units) * 2
                pgc = [0]
                cic = [0]
                cac = [0]
                half_a = {}

                def stepA(H):
                    e, tt = units[H // 2]
                    sl = e % 2
                    if tt == 0 and H % 2 == 0 and e == 2:
                        nxt = next_mixer_weights
                        if nxt is not None:
                            load_mixer_weights(nxt)
                    subs = []
                    for j in range(2):
                        s = tt * 4 + (H % 2) * 2 + j
                        ss = slice(s * 128, (s + 1) * 128)
                        bi = pgc[0] % 4
                        pgc[0] += 1
                        pg, kg = banks[bi], "ps%d" % bi
                        for kc in range(8):
                            op("pe", lambda t: t.matmul(pg[:], lhsT=h2T[:, kc, ss], rhs=Wgu[sl][:, kc, :], start=(kc == 0), stop=False), reads=["h2T", "Wgu%d" % sl], writes=[kg])
                        op("pe", lambda t: t.matmul(pg[:], lhsT=ones_bf[0:2, :], rhs=bgu[sl][0:2, :], start=False, stop=True), reads=["ones_bf", "bgu%d" % sl], writes=[kg])
                        c3 = cic[0] % NCH
                        cic[0] += 1
                        c4 = cac[0] % 4
                        cac[0] += 1
                        subs.append((s, pg, kg, c3, c4))
                    for (s, pg, kg, c3, c4) in subs:
                        op("dve", lambda v: v.tensor_scalar(out=cg[c3], in0=pg[:, 0:256], scalar1=7.0, scalar2=None, op0=ALU.min), reads=[kg], writes=["cg%d" % c3])
                    for (s, pg, kg, c3, c4) in subs:
                        op("act", lambda a: a.activation(out=csig[c3], in_=cg[c3], func=AF.Sigmoid, scale=1.702), reads=["cg%d" % c3], writes=["cs%d" % c3])
                    for (s, pg, kg, c3, c4) in subs:
                        op("dve", lambda v: v.tensor_scalar(out=cu[c3], in0=pg[:, 256:512], scalar1=8.0, scalar2=-6.0, op0=ALU.min, op1=ALU.max), reads=[kg], writes=["cu%d" % c3])
                    for (s, pg, kg, c3, c4) in subs:
                        op("dve", lambda v: v.scalar_tensor_tensor(out=cp_[c3], in0=cg[c3], scalar=wgt[:, s, e:e + 1], in1=cu[c3], op0=ALU.mult, op1=ALU.mult),
                           reads=["cg%d" % c3, "cu%d" % c3, "wgt"], writes=["cp%d" % c3])
                    for (s, pg, kg, c3, c4) in subs:
                        op("pool", lambda g: g.tensor_tensor(out=cact[c4], in0=cp_[c3], in1=csig[c3], op=ALU.mult), reads=["cp%d" % c3, "cs%d" % c3], writes=["ca%d" % c4])
                    half_a[H] = [(s, c4) for (s, pg, kg, c3, c4) in subs]
                    if tt == NT - 1 and H % 2 == 1 and e + 2 < E:
                        load_gu(e + 2)

                def stepC(H):
                    e, tt = units[H // 2]
                    t_idx = H // 2
                    bi = 4 + t_idx % 2
                    pT_b = banks[bi][:].bitcast(BF16)
                    kT = "ps%d" % bi
                    for (s, c4) in half_a.pop(H):
                        s4 = s % 4
                        for fc in range(2):
                            op("pe", lambda t: t.transpose(out=pT_b[:, fc * TE + s4 * 128:fc * TE + (s4 + 1) * 128], in_=cact[c4][:, fc * 128:(fc + 1) * 128], identity=identb[:]),
                               reads=["ca%d" % c4, "identb"], writes=[kT])
                    if H % 2 == 1:
                        ai = t_idx % 2
                        op("act", lambda a: a.activation(out=actT[ai].rearrange("p k t -> p (k t)"), in_=pT_b[:, 0:2 * TE], func=AF.Identity), reads=[kT], writes=["actT%d" % ai])

                ydc = [0]

                def stepD(t_idx):
                    e, tt = units[t_idx]
                    sl = e % 2
                    ai = t_idx % 2
                    for dc in range(8):
                        bi = 6 + ydc[0] % 2
                        ydc[0] += 1
                        pyd, kyd = banks[bi], "ps%d" % bi
                        for fc in range(2):
                            op("pe", lambda t: t.matmul(pyd[:], lhsT=Wd[sl][:, fc, dc * 128:(dc + 1) * 128], rhs=actT[ai][:, fc, :], start=(fc == 0), stop=(fc == 1)),
                               reads=["Wd%d" % sl, "actT%d" % ai], writes=[kyd])
                        xk = "XT%d_%d" % (tt, dc)
                        op("dve", lambda v: v.scalar_tensor_tensor(out=XT[:, dc, tt * TE:(tt + 1) * TE], in0=pyd[:], scalar=col(G2, dc), in1=XT[:, dc, tt * TE:(tt + 1) * TE], op0=ALU.mult, op1=ALU.add),
                           reads=[kyd, "dsc", xk], writes=[xk])
                    if tt == NT - 1 and e + 2 < E:
                        load_d(e + 2)

                load_gu(0)
                load_d(0)
                load_gu(1)
                load_d(1)
                for H in range(nH + 3):
                    if H < nH:
                        stepA(H)
                    if 1 <= H <= nH:
                        stepC(H - 1)
                    if H >= 3 and H % 2 == 1 and (H - 3) // 2 < len(units):
                        stepD((H - 3) // 2)
                for tt in range(UNIT // TE):
                    for dc in range(8):
                        pass
                P.barrier()
                for tq in range(UNIT // TM):
                    ln_tile(slice(tq * TM, (tq + 1) * TM), TM, V_LN2G + l * 8, V_LN2B + l * 8, None)

            P.barrier()
            for c in range(UNIT // 128):
                for g4 in range(2):
                    pb, pk = psn()
                    for q in range(4):
                        kc = g4 * 4 + q
                        op("pe", lambda t: t.transpose(out=pb[:, q * 128:(q + 1) * 128], in_=XT[:, kc, c * 128:(c + 1) * 128], identity=ident), reads=["XT", "CT"], writes=[pk])
                    op("act", lambda a: a.activation(out=xstage[:, g4 * 512:(g4 + 1) * 512], in_=pb[:], func=AF.Identity), reads=[pk], writes=["xstage"])
                dma("sp", "xout", [(out_d[b, tok0 + c * 128:tok0 + (c + 1) * 128, :], xstage)], reads=["xstage"])
    P.finish("sp")
    print("instructions", P.ninst, "waits", P.nwait, "arena mix/moe", mix_end * 4, moe_end * 4)
    es.close()
    return nc


_CONSTS = None


def _prep(inputs, NB, cores):
    global _CONSTS
    if _CONSTS is None:
        _CONSTS = _consts()
    g = lambda k: np.asarray(inputs[k])
    L = DEPTH
    perm = _win_perm()
    w_in = np.ascontiguousarray(g("w_in")[:, :, perm])
    vecs = np.zeros((NVEC, 128), np.float32)
    vecs[V_BADA:V_BADA + 192] = g("b_ada").reshape(L * 48, 128)
    vecs[V_LN1G:V_LN1G + 32] = g("ln1_g").reshape(L * 8, 128)
    vecs[V_LN1B:V_LN1B + 32] = g("ln1_b").reshape(L * 8, 128)
    vecs[V_LN2G:V_LN2G + 32] = g("ln2_g").reshape(L * 8, 128)
    vecs[V_LN2B:V_LN2B + 32] = g("ln2_b").reshape(L * 8, 128)
    vecs[V_BGATE:V_BGATE + 96] = g("b_gate").reshape(L * 3 * 8, 128)
    vecs[V_CONVW:V_CONVW + 24] = g("conv_w").reshape(L * 3 * 2, 128)
    shared = {
        "w_ada": g("w_ada"), "w_in": w_in, "vecs": vecs, "consts": _CONSTS,
        "gmlp_ln_g": g("gmlp_ln_g"), "gmlp_ln_b": g("gmlp_ln_b"),
        "w_spatial": g("w_spatial"), "b_spatial": g("b_spatial"),
        "w_branch": np.ascontiguousarray(g("w_branch").reshape(L, 768, D)),
        "w_gate_up": np.ascontiguousarray(g("w_gate_up").reshape(L, 384, D)),
        "w_out": g("w_out"), "w_router": g("w_router"), "b_router": g("b_router"),
        "w_gu": g("w_gu"), "b_gu": g("b_gu"), "w_down": g("w_down"), "b_down": g("b_down"),
        "ind": np.concatenate([np.zeros((1, DFF), np.float32), np.ones((1, DFF), np.float32)], axis=1),
    }
    x = g("x")
    c = g("c")
    pos = g("positions").astype(np.int32)
    maps = []
    for i in range(cores):
        bs = slice(i * NB, (i + 1) * NB)
        cT = np.ascontiguousarray(c[bs].reshape(NB, 8, 128).transpose(2, 1, 0))
        m = dict(shared)
        m.update({"x": np.ascontiguousarray(x[bs]), "cT": cT, "pos": np.ascontiguousarray(pos[bs])})
        maps.append(m)
    return maps


def kernel(**inputs):
    NB = 32 // NCORES
    nc = build(NB=NB, NL=DEPTH)
    maps = _prep(inputs, NB, NCORES)
    res = run_bass_kernel_spmd(nc, maps, core_ids=list(range(NCORES)))
    out = np.concatenate([np.asarray(r["out"]) for r in res.results], axis=0)
    return out.astype(np.float32)
```
